# Optimizing a Trainium2 kernel written in Bass

```python
import numpy as np
import jax, jax.numpy as jnp
from jax import lax

D_MODEL = 1024
BATCH = 32
SEQ = 2048
DEPTH = 2

D_HEAD = 64
ATT_DIM = 3 * D_MODEL // 8
ATT_HEADS = ATT_DIM // D_HEAD
GLA_DV = 64
GLA_DK = 32
GLA_VDIM = 3 * D_MODEL // 8
GLA_HEADS = GLA_VDIM // GLA_DV
GLA_KDIM = GLA_HEADS * GLA_DK
GLA_RANK = 16
GLA_TAU = 16.0
GLA_CHUNK = 64
SG_DIM = D_MODEL - ATT_DIM - GLA_VDIM
SG_GROUP_DIM = 64
SG_GROUPS = SG_DIM // SG_GROUP_DIM
SG_CHUNK = 128
MIX_DIM = ATT_DIM + GLA_VDIM + SG_DIM
DILATIONS = ((128, 1), (512, 4), (2048, 16))
ATT_BLOCK = 128
ROPE_THETA = 500000.0
ROPE_DIM = D_HEAD // 4
D_FF = 4 * D_MODEL
EPS = 1e-6
MASK_VALUE = -1e30
IN_SPLITS = (ATT_DIM, ATT_DIM, ATT_DIM, GLA_KDIM, GLA_KDIM, GLA_VDIM, GLA_VDIM, GLA_RANK, 2 * SG_DIM)
IN_DIM = sum(IN_SPLITS)

kernel_name = "hymba_style_gla_gmlp_dilated_hybrid"


def rms_norm(x, g):
    xf = x.astype(jnp.float32)
    y = xf * lax.rsqrt(jnp.mean(xf * xf, axis=-1, keepdims=True) + EPS)
    return (y * g.astype(jnp.float32)).astype(x.dtype)


def layer_norm(x, g, b):
    xf = x.astype(jnp.float32)
    mu = jnp.mean(xf, axis=-1, keepdims=True)
    xc = xf - mu
    y = xc * lax.rsqrt(jnp.mean(xc * xc, axis=-1, keepdims=True) + EPS)
    return (y * g.astype(jnp.float32) + b.astype(jnp.float32)).astype(x.dtype)


def rope_tables(seq):
    inv = ROPE_THETA ** (-jnp.arange(0, ROPE_DIM, 2, dtype=jnp.float32) / ROPE_DIM)
    ang = jnp.arange(seq, dtype=jnp.float32)[:, None] * inv[None, :]
    return jnp.cos(ang), jnp.sin(ang)


def partial_rope(x, cos, sin):
    half = ROPE_DIM // 2
    c = cos[None, :, None, :].astype(x.dtype)
    s = sin[None, :, None, :].astype(x.dtype)
    x1, x2, rest = x[..., :half], x[..., half:ROPE_DIM], x[..., ROPE_DIM:]
    return jnp.concatenate([x1 * c - x2 * s, x2 * c + x1 * s, rest], axis=-1)


def dilated_branch(q, k, v, window, dilation):
    B, S, H, Dh = q.shape
    n = S // dilation
    nb = -(-n // ATT_BLOCK)
    pad = nb * ATT_BLOCK - n
    span = window // dilation

    def to_sub(t):
        t = t.reshape(B, n, dilation, H, Dh).transpose(0, 2, 3, 1, 4)
        t = jnp.pad(t, ((0, 0), (0, 0), (0, 0), (0, pad), (0, 0)))
        return t.reshape(B, dilation, H, nb, ATT_BLOCK, Dh)

    def with_prev(t):
        prev = jnp.pad(t, ((0, 0), (0, 0), (0, 0), (1, 0), (0, 0), (0, 0)))[:, :, :, :-1]
        return jnp.concatenate([prev, t], axis=4)

    qs = to_sub(q)
    kb = with_prev(to_sub(k))
    vb = with_prev(to_sub(v))
    s = jnp.einsum('bdhnqc,bdhnkc->bdhnqk', qs, kb).astype(jnp.float32)
    qi = jnp.arange(ATT_BLOCK)[:, None] + ATT_BLOCK
    kj = jnp.arange(2 * ATT_BLOCK)[None, :]
    dist = qi - kj
    kpos = jnp.arange(nb)[:, None, None] * ATT_BLOCK + kj[None] - ATT_BLOCK
    valid = (dist >= 0) & (dist <= span) & (kpos >= 0)
    s = jnp.where(valid, s, MASK_VALUE)
    lse = jax.nn.logsumexp(s, axis=-1)
    p = jnp.exp(s - lse[..., None]).astype(v.dtype)
    o = jnp.einsum('bdhnqk,bdhnkc->bdhnqc', p, vb)
    o = o.reshape(B, dilation, H, nb * ATT_BLOCK, Dh)[:, :, :, :n]
    o = o.transpose(0, 3, 1, 2, 4).reshape(B, S, H, Dh)
    lse = lse.reshape(B, dilation, H, nb * ATT_BLOCK)[:, :, :, :n]
    lse = lse.transpose(0, 3, 1, 2).reshape(B, S, H)
    return o, lse


def dilated_attention(q, k, v, cos, sin, q_norm_g, k_norm_g):
    q = partial_rope(rms_norm(q, q_norm_g), cos, sin) * (D_HEAD ** -0.5)
    k = partial_rope(rms_norm(k, k_norm_g), cos, sin)
    outs, lses = [], []
    for window, dilation in DILATIONS:
        o, l = dilated_branch(q, k, v, window, dilation)
        outs.append(o)
        lses.append(l)
    wts = jax.nn.softmax(jnp.stack(lses, axis=0), axis=0).astype(v.dtype)
    o = jnp.einsum('rbsh,rbshc->bshc', wts, jnp.stack(outs, axis=0))
    B, S = o.shape[:2]
    return o.reshape(B, S, ATT_DIM)


def gated_linear_attention(q, k, v, r, a_lr, w_a2, b_a, g_norm):
    B, S = q.shape[:2]
    C = GLA_CHUNK
    N = S // C
    f32 = jnp.float32
    gk = jax.nn.log_sigmoid((a_lr @ w_a2 + b_a).astype(f32)) / GLA_TAU
    bcum = jnp.cumsum(gk.reshape(B, N, C, GLA_HEADS, GLA_DK), axis=2)
    qf = q.astype(f32).reshape(B, N, C, GLA_HEADS, GLA_DK) * (GLA_DK ** -0.5)
    kf = k.astype(f32).reshape(B, N, C, GLA_HEADS, GLA_DK)
    vf = v.astype(f32).reshape(B, N, C, GLA_HEADS, GLA_DV)
    b_last = bcum[:, :, -1:]
    q_t = qf * jnp.exp(bcum)
    k_t = kf * jnp.exp(-bcum)
    k_end = kf * jnp.exp(b_last - bcum)
    causal = jnp.tril(jnp.ones((C, C), dtype=bool))
    A = jnp.where(causal, jnp.einsum('bnthk,bnshk->bnhts', q_t, k_t), 0.0)
    o_intra = jnp.einsum('bnhts,bnshv->bnthv', A, vf)
    kv = jnp.einsum('bnshk,bnshv->nbhkv', k_end, vf)
    decay = jnp.exp(b_last[:, :, 0]).transpose(1, 0, 2, 3)

    def step(state, inp):
        dec, kv_n = inp
        return state * dec[..., None] + kv_n, state

    _, states = lax.scan(step, jnp.zeros((B, GLA_HEADS, GLA_DK, GLA_DV), f32), (decay, kv))
    o_inter = jnp.einsum('bnthk,nbhkv->bnthv', q_t, states)
    o = (o_intra + o_inter).reshape(B, S, GLA_HEADS, GLA_DV)
    o = rms_norm(o, g_norm) * jax.nn.silu(r.astype(f32))
    return o.reshape(B, S, GLA_VDIM).astype(q.dtype)


def spatial_gating(z, ln_g, ln_b, w_s, b_s):
    B, S, _ = z.shape
    N = S // SG_CHUNK
    z = jax.nn.gelu(z)
    u, v = jnp.split(z, 2, axis=-1)
    v = layer_norm(v, ln_g, ln_b).reshape(B, N, SG_CHUNK, SG_GROUPS, SG_GROUP_DIM)
    causal = jnp.tril(jnp.ones((SG_CHUNK, SG_CHUNK), dtype=bool))
    w = jnp.where(causal, w_s, jnp.zeros_like(w_s))
    s = jnp.einsum('gts,bnsgc->bntgc', w, v) + b_s.T[None, None, :, :, None]
    return u * s.reshape(B, S, SG_DIM)


def hybrid_mixer(h, cos, sin, w_in, q_norm_g, k_norm_g, gla_w_a2, gla_b_a, gla_norm_g,
                 sg_ln_g, sg_ln_b, sg_w, sg_b, w_out):
    B, S, _ = h.shape
    proj = h @ w_in
    split_points = np.cumsum(IN_SPLITS)[:-1].tolist()
    q_a, k_a, v_a, q_g, k_g, v_g, r_g, a_g, z_s = jnp.split(proj, split_points, axis=-1)
    o_att = dilated_attention(q_a.reshape(B, S, ATT_HEADS, D_HEAD), k_a.reshape(B, S, ATT_HEADS, D_HEAD),
                              v_a.reshape(B, S, ATT_HEADS, D_HEAD), cos, sin, q_norm_g, k_norm_g)
    o_gla = gated_linear_attention(q_g, k_g, v_g.reshape(B, S, GLA_HEADS, GLA_DV),
                                   r_g.reshape(B, S, GLA_HEADS, GLA_DV), a_g, gla_w_a2, gla_b_a, gla_norm_g)
    o_sg = spatial_gating(z_s, sg_ln_g, sg_ln_b, sg_w, sg_b)
    return jnp.concatenate([o_att, o_gla, o_sg], axis=-1) @ w_out


def squared_relu_mlp(h, w1, w2):
    return jnp.square(jax.nn.relu(h @ w1)) @ w2


def setup_inputs(seed: int = 0) -> dict:
    key = jax.random.key(seed)
    ks = jax.random.split(key, 18)
    nrm = lambda k, shape, scale: jax.random.normal(k, shape, jnp.float32) * scale
    return {
        "x": nrm(ks[0], (BATCH, SEQ, D_MODEL), 1.0),
        "norm1_g": 1.0 + nrm(ks[1], (DEPTH, D_MODEL), 0.02),
        "w_in": nrm(ks[2], (DEPTH, D_MODEL, IN_DIM), D_MODEL ** -0.5),
        "q_norm_g": 1.0 + nrm(ks[3], (DEPTH, D_HEAD), 0.02),
        "k_norm_g": 1.0 + nrm(ks[4], (DEPTH, D_HEAD), 0.02),
        "gla_w_a2": nrm(ks[5], (DEPTH, GLA_RANK, GLA_KDIM), GLA_RANK ** -0.5),
        "gla_b_a": nrm(ks[6], (DEPTH, GLA_KDIM), 0.1),
        "gla_norm_g": 1.0 + nrm(ks[7], (DEPTH, GLA_DV), 0.02),
        "sg_ln_g": 1.0 + nrm(ks[8], (DEPTH, SG_DIM), 0.02),
        "sg_ln_b": nrm(ks[9], (DEPTH, SG_DIM), 0.02),
        "sg_w": nrm(ks[10], (DEPTH, SG_GROUPS, SG_CHUNK, SG_CHUNK), SG_CHUNK ** -0.5),
        "sg_b": 1.0 + nrm(ks[11], (DEPTH, SG_GROUPS, SG_CHUNK), 0.02),
        "w_out": nrm(ks[12], (DEPTH, MIX_DIM, D_MODEL), MIX_DIM ** -0.5),
        "norm2_g": 1.0 + nrm(ks[13], (DEPTH, D_MODEL), 0.02),
        "w_ff1": nrm(ks[14], (DEPTH, D_MODEL, D_FF), D_MODEL ** -0.5),
        "w_ff2": nrm(ks[15], (DEPTH, D_FF, D_MODEL), D_FF ** -0.5),
    }


def reference(x, norm1_g, w_in, q_norm_g, k_norm_g, gla_w_a2, gla_b_a, gla_norm_g,
              sg_ln_g, sg_ln_b, sg_w, sg_b, w_out, norm2_g, w_ff1, w_ff2):
    cos, sin = rope_tables(x.shape[1])
    for l in range(DEPTH):
        h = rms_norm(x, norm1_g[l])
        x = x + hybrid_mixer(h, cos, sin, w_in[l], q_norm_g[l], k_norm_g[l], gla_w_a2[l], gla_b_a[l],
                             gla_norm_g[l], sg_ln_g[l], sg_ln_b[l], sg_w[l], sg_b[l], w_out[l])
        x = x + squared_relu_mlp(rms_norm(x, norm2_g[l]), w_ff1[l], w_ff2[l])
    return x
```

```python
import numpy as np
from contextlib import ExitStack
import concourse.bass as bass
import concourse.mybir as mybir
from concourse.bass_utils import run_bass_kernel_spmd

F32 = mybir.dt.float32
BF16 = mybir.dt.bfloat16
AF = mybir.ActivationFunctionType
ALU = mybir.AluOpType
AX = mybir.AxisListType

D = 1024
S = 2048
NB = 16
DEPTH = 2
NCORES = 8
SEQ_PER_CORE = 4
IN_DIM = 2832
DFF = 4096
EPS = 1e-6
OQ, OK_, OV, OQG, OKG, OVG, ORG, OAG, OZS = 0, 384, 768, 1152, 1344, 1536, 1920, 2304, 2320
HS = 66


class Res:
    __slots__ = ("name", "w", "rs", "extra")

    def __init__(self, name):
        self.name = name
        self.w = None
        self.rs = []
        self.extra = None


class Op:
    __slots__ = ("eng", "fn", "deps", "odeps", "sig", "cnt", "dkey", "dval", "isdma", "ndma", "idx", "dur", "nbytes",
                 "succ", "nleft", "rt", "fin")


SEM_LAT = 150.0
DMA_BW = 270.0
DMA_LAT = 2000.0


class Prog:
    ENGS = ("pe", "act", "dve", "pool", "sp")

    use_bl = True

    def __init__(self):
        self.all = []
        self.nops = 0
        self.dma_keys = set()

    def add(self, eng, fn, reads=(), writes=(), dkey=None, ndma=1, dur=300.0, nbytes=0):
        o = Op()
        o.eng = eng
        o.fn = fn
        o.sig = False
        o.cnt = 0
        o.isdma = dkey is not None
        o.dkey = dkey
        o.ndma = ndma
        o.dur = dur
        o.nbytes = nbytes
        o.idx = len(self.all)
        deps = {}

        def need(p, kind):
            if p is None or p is o:
                return
            ent = deps.get(id(p))
            if ent is None:
                deps[id(p)] = (p, {kind})
            else:
                ent[1].add(kind)

        for r in reads:
            need(r.w, "RAW")
        for w in writes:
            need(w.w, "WAW")
            for rd in w.rs:
                need(rd, "WAR")
            if w.extra:
                for p in w.extra:
                    need(p, "WAR")
                w.extra = None
        sync, order = [], []
        for p, kinds in deps.values():
            if p.isdma:
                sync.append(p)
            elif p.eng == eng:
                if eng != "pe" and ("RAW" in kinds or eng == "pool"):
                    sync.append(p)
                    p.sig = True
                else:
                    order.append(p)
            else:
                sync.append(p)
                p.sig = True
        o.deps = sync
        o.odeps = order
        for r in reads:
            r.rs.append(o)
        for w in writes:
            w.w = o
            w.rs = []
        if o.isdma:
            self.dma_keys.add(dkey)
        self.all.append(o)
        self.nops += 1
        return o

    def phase_switch(self, from_res, to_res):
        seen = {}
        for r in from_res:
            if r.w is not None:
                seen[id(r.w)] = r.w
            for p in r.rs:
                seen[id(p)] = p
        L = list(seen.values())
        for r in to_res:
            r.extra = (r.extra or []) + L

    def schedule(self, enable=True):
        ops = self.all
        self.ops = {e: [] for e in self.ENGS}
        if not enable:
            for o in ops:
                self.ops[o.eng].append(o)
            self.makespan = 0.0
            return
        for o in ops:
            o.succ = []
            o.rt = 0.0
            o.fin = None
        for o in ops:
            n = 0
            for p in o.deps:
                p.succ.append((o, True))
                n += 1
            for p in o.odeps:
                p.succ.append((o, False))
                n += 1
            o.nleft = n
        bl = [0.0] * len(ops)
        for o in reversed(ops):
            m = 0.0
            for (q, issync) in o.succ:
                v = bl[q.idx] + (SEM_LAT if issync else 0.0)
                if v > m:
                    m = v
            d = o.dur if not o.isdma else (o.nbytes / DMA_BW + DMA_LAT)
            bl[o.idx] = m + d
        ready = {e: [] for e in self.ENGS}
        for o in ops:
            if o.nleft == 0:
                ready[o.eng].append(o)
        free = {e: 0.0 for e in self.ENGS}
        dma_free = 0.0
        left = len(ops)
        while left:
            best = None
            bkey = None
            for e in self.ENGS:
                fe = free[e]
                for o in ready[e]:
                    st = o.rt if o.rt > fe else fe
                    key = (st, -bl[o.idx] if self.use_bl else o.idx, o.idx)
                    if bkey is None or key < bkey:
                        bkey = key
                        best = o
            o = best
            st = bkey[0]
            ready[o.eng].remove(o)
            if o.isdma:
                free[o.eng] = st + 60.0 * o.ndma
                xs = max(st + 60.0, dma_free)
                dma_free = xs + o.nbytes / DMA_BW
                o.fin = dma_free + DMA_LAT
            else:
                o.fin = st + o.dur
                free[o.eng] = o.fin
            self.ops[o.eng].append(o)
            for (q, issync) in o.succ:
                t = o.fin + (SEM_LAT if (issync and (q.eng != o.eng or o.isdma)) else (60.0 if issync else 0.0))
                if t > q.rt:
                    q.rt = t
                q.nleft -= 1
                if q.nleft == 0:
                    ready[q.eng].append(q)
            left -= 1
        self.makespan = max(o.fin for o in ops)

    def emit(self, nc, stack):
        esem = {}
        for e in ("pe", "act", "dve", "pool"):
            esem[e] = stack.enter_context(nc.semaphore("s_" + e))
        dsem = {}
        for k in sorted(self.dma_keys):
            dsem[k] = stack.enter_context(nc.semaphore("d_" + str(k)))
        tot = {}
        for e in self.ENGS:
            for o in self.ops[e]:
                if o.isdma:
                    tot[o.dkey] = tot.get(o.dkey, 0) + 16 * o.ndma
                    o.dval = tot[o.dkey]
        for e in ("pe", "act", "dve", "pool"):
            c = 0
            for o in self.ops[e]:
                if o.sig and not o.isdma:
                    c += 1
                    o.cnt = c
        self.sig_counts = {e: sum(1 for o in self.ops[e] if o.sig and not o.isdma) for e in esem}

        def run(engname, e):
            known = {}
            for o in self.ops[engname]:
                waits = {}
                for p in o.deps:
                    if p.isdma:
                        s, v = dsem[p.dkey], p.dval
                    else:
                        s, v = esem[p.eng], p.cnt
                    if known.get(s, 0) >= v:
                        continue
                    if waits.get(s, (None, 0))[1] < v:
                        waits[s] = (s, v)
                for s, v in waits.values():
                    e.wait_ge(s, v)
                    known[s] = v
                if o.isdma:
                    o.fn(e, dsem[o.dkey])
                else:
                    ins = o.fn(e)
                    if o.sig:
                        ins.then_inc(esem[engname], 1)

        block = stack.enter_context(nc.Block())

        @block.tensor
        def _(e):
            run("pe", e)

        @block.scalar
        def _(e):
            run("act", e)

        @block.vector
        def _(e):
            run("dve", e)

        @block.gpsimd
        def _(e):
            run("pool", e)

        @block.sync
        def _(e):
            run("sp", e)


class _Dummy:
    def then_inc(self, *a, **k):
        return self


class _Rec:
    def __init__(self):
        self.calls = []

    def __getattr__(self, name):
        def f(*a, **k):
            self.calls.append((name, a, k))
            return _Dummy()
        return f


class Builder:
    def __init__(self, nseq=SEQ_PER_CORE, nlayers=DEPTH, stage=99, dbg_cols=0, tt=512, sched=True):
        self.sched = sched
        self.NE = 3
        self.NCC = 1
        self.NF = (1, 1, 1)
        self.nseq = nseq
        self.nlayers = nlayers
        self.stage = stage
        self.dbg_cols = dbg_cols
        self.dbg_off = 0
        self.taps = {}
        self.TT = tt
        self.nc = bass.Bass("TRN2", target_bir_lowering=False, dynamic_dma_scratch_size=256)
        self.P = Prog()
        self.stack = ExitStack()
        self.uid = 0
        self.rr = 0
        self.tapi = 0

    def sb(self, name, shape, dt):
        return self.stack.enter_context(self.nc.sbuf_tensor(name, list(shape), dt))

    def dram(self, name, shape, dt, kind):
        return self.nc.dram_tensor(name, list(shape), dt, kind=kind).ap()

    def res(self, name):
        return Res(name)

    def est(self, eng, fn, isdma=False):
        rec = _Rec()
        if isdma:
            fn(rec, None)
        else:
            fn(rec)
        dur = 0.0
        nbytes = 0
        for (name, a, k) in rec.calls:
            if name == "matmul":
                rhs = a[2] if len(a) > 2 else k["rhs"]
                n = int(np.prod(rhs.shape[1:]))
                if rhs.dtype == F32:
                    d = max(60.0, 2.0 * n)
                elif n >= 300:
                    d = 246.0
                else:
                    d = max(64.0, 0.44 * n + 20.0)
                dur += d
            elif name == "transpose":
                dur += 86.0
            elif name == "activation":
                n = int(np.prod(k["in_"].shape[1:]))
                dur += 150.0 + 0.66 * n + (100.0 if k.get("accum_out") is not None else 0.0)
            elif name == "dma_start":
                o = k["out"]
                nbytes += int(np.prod(o.shape)) * mybir.dt.size(o.dtype)
            elif name == "nop":
                dur += 50.0
            else:
                o = k.get("out", a[0] if a else None)
                n = int(np.prod(o.shape[1:])) if o is not None else 64
                if eng == "pool":
                    dur += 1000.0 if k.get("op") == ALU.pow else 150.0 + 1.8 * n
                else:
                    dur += 150.0 + 1.0 * n
        return dur, nbytes

    def pe(self, fn, reads, writes):
        return self.P.add("pe", fn, reads, writes, dur=self.est("pe", fn)[0])

    def act(self, fn, reads, writes):
        return self.P.add("act", fn, reads, writes, dur=self.est("act", fn)[0])

    def dve(self, fn, reads, writes):
        return self.P.add("dve", fn, reads, writes, dur=self.est("dve", fn)[0])

    def pool(self, fn, reads, writes):
        return self.P.add("pool", fn, reads, writes, dur=self.est("pool", fn)[0])

    def dma(self, key, pairs, reads, writes):
        def fn(e, sem, pairs=pairs):
            for (o, i) in pairs:
                e.dma_start(out=o, in_=i).then_inc(sem, 16)
        return self.P.add("sp", fn, reads, writes, dkey=key, ndma=len(pairs), nbytes=self.est("sp", fn, True)[1])

    def bank(self):
        k = self.rr % 6
        self.rr += 1
        return k

    def tap(self, name, ap, res, ncols, parts=128):
        if self.dbg_cols == 0:
            return
        off = self.dbg_off
        self.dbg_off += ncols
        assert self.dbg_off <= self.dbg_cols, self.dbg_off
        self.taps[name] = (off, ncols, parts)
        for c0 in range(0, ncols, 512):
            n = min(512, ncols - c0)
            i = self.tapi % 2
            self.tapi += 1
            st, r = self.RELU[i], self.RELUr[i]
            self.pool(lambda e, st=st, c0=c0, n=n: e.tensor_copy(out=st[:, 0:n], in_=ap[:, c0:c0 + n]), [res], [r])
            self.dma("tap%d" % i, [(self.dbg[:, off + c0:off + c0 + n], st[:, 0:n])], [r], [self.res("dbgd_" + name)])

    def build(self):
        nc = self.nc
        nseq, L = self.nseq, self.nlayers
        self.x_in = self.dram("x", [nseq, S, D], F32, "ExternalInput")
        self.y_out = self.dram("y", [nseq, S, D], F32, "ExternalOutput")
        self.w_in_f = self.dram("w_in", [DEPTH, D, IN_DIM], F32, "ExternalInput")
        self.w_out_f = self.dram("w_out", [DEPTH, D, D], F32, "ExternalInput")
        self.w_ff1_f = self.dram("w_ff1", [DEPTH, D, DFF], F32, "ExternalInput")
        self.w_ff2_f = self.dram("w_ff2", [DEPTH, DFF, D], F32, "ExternalInput")
        self.gT_d = self.dram("gT", [DEPTH, 128, 16], F32, "ExternalInput")
        self.bc_d = self.dram("bc", [DEPTH, 128, 704], F32, "ExternalInput")
        self.wa2_d = self.dram("wa2", [DEPTH, 16, 192], F32, "ExternalInput")
        self.ba_d = self.dram("ba", [DEPTH, 1, 192], F32, "ExternalInput")
        self.sgw_d = self.dram("sgwT", [DEPTH, 128, 4, 128], F32, "ExternalInput")
        self.sgb_d = self.dram("sgbT", [DEPTH, 128, 4], F32, "ExternalInput")
        self.cmask_d = self.dram("c_mask", [128, 16 * 128], F32, "ExternalInput")
        self.cmisc_d = self.dram("c_misc", [128, 4 * 128], F32, "ExternalInput")
        self.crope_d = self.dram("c_rope", [128, 16 * 16], F32, "ExternalInput")
        if self.dbg_cols:
            self.dbg = self.dram("dbg", [128, self.dbg_cols], F32, "ExternalOutput")
        self.win_b = self.dram("win_b", [DEPTH, D, IN_DIM], BF16, "Internal")
        self.wout_b = self.dram("wout_b", [DEPTH, D, D], BF16, "Internal")
        self.wff1_b = self.dram("wff1_b", [DEPTH, D, DFF], BF16, "Internal")
        self.wff2_b = self.dram("wff2_b", [DEPTH, DFF, D], BF16, "Internal")

        self.X = self.sb("X", [128, NB, D], F32 if not getattr(self, "simshrink", False) else BF16)
        self.Xr = [self.res("X%d" % b) for b in range(NB)]
        self.WIN = self.sb("WIN", [128, 8, IN_DIM], BF16)
        self.WINr = self.res("WIN")
        REG_ELEMS = 28672
        self.REG = self.sb("REG", [128, REG_ELEMS], BF16)
        R = self.REG
        o = 0
        self.QKT = R[:, o:o + 6 * S].rearrange("p (j t) -> p j t", j=6); o += 6 * S
        self.VAUG = R[:, o:o + NB * 6 * HS].rearrange("p (b h c) -> p b h c", b=NB, h=6); o += NB * 6 * HS
        self.WOUT = R[:, o:o + 8 * D].rearrange("p (k c) -> p k c", k=8); o += 8 * D
        self.CT = R[:, o:o + 8 * 128].rearrange("p (k t) -> p k t", k=8); o += 8 * 128
        self.QKN = R[:, o:o + 768]; o += 768
        assert o <= REG_ELEMS, o
        TT = self.TT
        o = 0
        self.H2Ts = [R[:, o + i * 8 * TT:o + (i + 1) * 8 * TT].rearrange("p (k t) -> p k t", k=8) for i in range(2)]; o += 2 * 8 * TT
        self.H1T = [R[:, o + i * 4 * TT:o + (i + 1) * 4 * TT].rearrange("p (k t) -> p k t", k=4) for i in range(2)]; o += 2 * 4 * TT
        self.W1 = [R[:, o + i * 4096:o + (i + 1) * 4096].rearrange("p (k c) -> p k c", k=8) for i in range(2)]; o += 2 * 4096
        self.W2 = [R[:, o + i * 4096:o + (i + 1) * 4096].rearrange("p (k c) -> p k c", k=4) for i in range(2)]; o += 2 * 4096
        assert o <= REG_ELEMS, o
        self.QKTr = [self.res("QKT%d" % b) for b in range(NB)]
        self.VAUGr = [self.res("VAUG%d" % b) for b in range(NB)]
        self.VONESr = self.res("VONES")
        self.WOUTr = self.res("WOUT")
        self.CTr = self.res("CT")
        self.QKNr = self.res("QKN")
        self.H2Trs = [self.res("H2T0"), self.res("H2T1")]
        self.H1Tr = [self.res("H1T0"), self.res("H1T1")]
        self.W1r = [self.res("W1_0"), self.res("W1_1")]
        self.W2r = [self.res("W2_0"), self.res("W2_1")]
        self.mixres = self.QKTr + self.VAUGr + [self.VONESr, self.WOUTr, self.CTr, self.QKNr]
        self.ffnres = self.H2Trs + self.H1Tr + self.W1r + self.W2r

        self.MASK = self.sb("MASK", [128, 16, 128], BF16)
        self.IDENT = self.sb("IDENT", [128, 128], BF16)
        self.CAUS = self.sb("CAUS", [128, 128], BF16)
        self.MISCF = self.sb("MISCF", [128, 3, 128], F32)
        self.ROPE = self.sb("ROPE", [128, 2, NB, 8], F32)
        self.Cr = self.res("consts")
        self.GT = self.sb("GT", [128, DEPTH, 16], F32)
        self.BC = self.sb("BC", [128, 704], F32); self.BCr = self.res("BC")
        self.WA2 = self.sb("WA2", [17, DEPTH, 192], F32)
        self.SGW = self.sb("SGW", [128, DEPTH, 4, 128], BF16)
        self.SGB = self.sb("SGB", [128, DEPTH, 4], F32)
        self.NEGH = self.sb("NEGH", [128, 16], F32)
        self.Pr = self.res("params")

        NF = self.NF
        self.FAs = [[self.sb("FA%d_%d" % (i, j), [128, n], F32) for i, n in enumerate((768, 768, 384))] for j in range(NF[0])]
        self.FArs = [[self.res("FA%d_%d" % (i, j)) for i in range(3)] for j in range(NF[0])]
        self.FGs = [[self.sb("FG%d_%d" % (i, j), [128, n], F32) for i, n in enumerate((384, 768, 384, 576, 768))] for j in range(NF[1])]
        self.FGrs = [[self.res("FG%d_%d" % (i, j)) for i in range(5)] for j in range(NF[1])]
        self.FSs = [[self.sb("FS%d_%d" % (i, j), [128, 512], F32) for i in range(2)] for j in range(NF[2])]
        self.FSrs = [[self.res("FS%d_%d" % (i, j)) for i in range(2)] for j in range(NF[2])]
        self.FA, self.FAr = self.FAs[0], self.FArs[0]
        self.HNs = [self.sb("HN%d" % i, [128, D], BF16) for i in range(2)]; self.HNrs = [self.res("HN0"), self.res("HN1")]
        self.HTs = [self.sb("HT%d" % i, [128, 8, 128], BF16) for i in range(2)]; self.HTrs = [self.res("HT0"), self.res("HT1")]
        self.hni = 0
        NE = self.NE
        self.E = [self.sb("E%d" % i, [128, 512], BF16) for i in range(NE)]
        self.Er = [self.res("E%d" % i) for i in range(NE)]
        self.CCs = [self.sb("CC%d" % i, [128, D], BF16) for i in range(self.NCC)]
        self.CCrs = [[self.res("CCatt%d" % i), self.res("CCgla%d" % i), self.res("CCsg%d" % i)] for i in range(self.NCC)]
        self.ST = self.sb("ST", [128, 64], F32); self.STr = [self.res("ST%d" % i) for i in range(16)]
        self.AGT = self.sb("AGT", [32, 128], F32); self.AGTr = self.res("AGT")
        self.GQK = self.sb("GQK", [128, 576], BF16); self.GQKr = self.res("GQK")
        self.GT2 = self.sb("GT2", [32, 12, 128], BF16); self.GT2r = self.res("GT2")
        self.VG = self.sb("VG", [128, 384], BF16); self.VGr = self.res("VG")
        self.AM = self.sb("AM", [128, 768], BF16); self.AMr = self.res("AM")
        self.SF = self.sb("SF", [32, 384], F32); self.SFr = self.res("SF")
        self.SB_ = self.sb("SBF", [32, 384], BF16); self.SBr = self.res("SBF")
        self.DEC = self.sb("DEC", [32, 6], F32); self.DECr = self.res("DEC")
        self.VLN = self.sb("VLN", [128, 256], BF16); self.VLNr = self.res("VLN")
        self.RELU = [self.FA[0], self.FA[1]]
        self.RELUr = [self.FAr[0], self.FAr[1]]

        self.PS = [self.stack.enter_context(nc.psum_tensor("ps%d" % i, [128, 512], F32)) for i in range(8)]
        self.PSr = [self.res("ps%d" % i) for i in range(8)]

        self.setup()
        if self.stage >= 1:
            self.convert_weights()
        self.marks = [("setup+convert", len(self.P.all))]
        for s in range(nseq):
            self.load_x(s)
            for l in range(L):
                self.mixer_phase(s, l)
                self.marks.append(("mix s%d l%d" % (s, l), len(self.P.all)))
                if self.stage >= 8:
                    self.ffn_phase(s, l)
                    self.marks.append(("ffn s%d l%d" % (s, l), len(self.P.all)))
            self.store_x(s)
        self.P.add("sp", lambda e: e.nop(), self.out_res, [], dur=50.0)
        self.P.schedule(self.sched)
        if getattr(self, "simshrink", False):
            self.stack.close()
            return None
        self.P.emit(nc, self.stack)
        self.stack.close()
        return nc

    def setup(self):
        stg = self.X
        sr = self.Xr
        self.out_res = []
        self.dma("c0", [(stg[:, 0, :], self.cmask_d[:, 0:1024]), (stg[:, 1, :], self.cmask_d[:, 1024:2048]),
                        (self.MISCF[:, :, :].rearrange("p a b -> p (a b)"), self.cmisc_d[:, 128:512]), (stg[:, 4, 0:128], self.cmisc_d[:, 0:128]),
                        (self.ROPE[:, :, :, :].rearrange("p a b c -> p (a b c)"), self.crope_d)],
                 [], [sr[0], sr[1], sr[4], self.Cr])
        self.dve(lambda e: e.tensor_copy(out=self.MASK[:, 0:8, :].rearrange("p a b -> p (a b)"), in_=stg[:, 0, :]), [sr[0]], [self.Cr])
        self.dve(lambda e: e.tensor_copy(out=self.MASK[:, 8:16, :].rearrange("p a b -> p (a b)"), in_=stg[:, 1, :]), [sr[1]], [self.Cr])
        self.dve(lambda e: e.tensor_copy(out=self.IDENT[:, :], in_=stg[:, 4, 0:128]), [sr[4]], [self.Cr])
        self.dve(lambda e: e.tensor_copy(out=self.CAUS[:, :], in_=self.MISCF[:, 0, :]), [self.Cr], [self.Cr])
        self.dve(lambda e: e.memset(self.NEGH[:, :], -0.5), [], [self.Cr])
        self.dve(lambda e: e.memset(self.AGT[:, :], 1.0), [], [self.AGTr])
        pairs = [(self.GT[:, l, :], self.gT_d[l]) for l in range(DEPTH)]
        pairs += [(self.WA2[0:16, l, :], self.wa2_d[l]) for l in range(DEPTH)]
        pairs += [(self.WA2[16:17, l, :], self.ba_d[l]) for l in range(DEPTH)]
        pairs += [(self.SGB[:, l, :], self.sgb_d[l]) for l in range(DEPTH)]
        pairs += [(stg[:, 2 + l, 0:512], self.sgw_d[l].rearrange("s g t -> s (g t)")) for l in range(DEPTH)]
        self.dma("c1", pairs, [], [self.Pr, sr[2], sr[3]])
        for l in range(DEPTH):
            self.dve(lambda e, l=l: e.tensor_tensor(
                out=self.SGW[:, l, :, :], in0=stg[:, 2 + l, 0:512].rearrange("p (g t) -> p g t", g=4),
                in1=self.MISCF[:, 0:1, :].to_broadcast([128, 4, 128]), op=ALU.mult), [sr[2 + l], self.Cr], [self.Pr])

    def convert_weights(self):
        jobs = []
        for l in range(self.nlayers):
            jobs.append((self.w_in_f[l], self.win_b[l], D * IN_DIM // 128, "cv_win%d" % l))
            jobs.append((self.w_out_f[l], self.wout_b[l], D * D // 128, "cv_wout%d" % l))
            jobs.append((self.w_ff1_f[l], self.wff1_b[l], D * DFF // 128, "cv_ff1%d" % l))
            jobs.append((self.w_ff2_f[l], self.wff2_b[l], DFF * D // 128, "cv_ff2%d" % l))
        self.wres = {}
        CH = 2048
        sin_ap = [self.X[:, 10 + 2 * i:12 + 2 * i, :].rearrange("p a b -> p (a b)") for i in range(2)]
        sin_r = [[self.Xr[10 + 2 * i], self.Xr[11 + 2 * i]] for i in range(2)]
        sout_ap = [self.X[:, 14 + i, :].bitcast(BF16) for i in range(2)]
        sout_r = [[self.Xr[14 + i]] for i in range(2)]
        k = 0
        engs = ["dve", "act", "pool"]
        for (src, dst, n, name) in jobs:
            sflat = src.rearrange("r c -> (r c)").rearrange("(p n) -> p n", p=128)
            dflat = dst.rearrange("r c -> (r c)").rearrange("(p n) -> p n", p=128)
            wr = self.res(name)
            self.wres[name] = wr
            off = 0
            while off < n:
                c = min(CH, n - off)
                i = k % 2
                si = sin_ap[i][:, 0:c]
                so = sout_ap[i][:, 0:c]
                self.dma("cvi%d" % i, [(si, sflat[:, off:off + c])], [], sin_r[i])
                en = engs[k % 3]
                if en == "act":
                    self.act(lambda e, so=so, si=si: e.activation(out=so, in_=si, func=AF.Copy), sin_r[i], sout_r[i])
                elif en == "dve":
                    self.dve(lambda e, so=so, si=si: e.tensor_copy(out=so, in_=si), sin_r[i], sout_r[i])
                else:
                    self.pool(lambda e, so=so, si=si: e.tensor_copy(out=so, in_=si), sin_r[i], sout_r[i])
                self.dma("cvo%d" % i, [(dflat[:, off:off + c], so)], sout_r[i], [wr])
                off += c
                k += 1

    def load_x(self, s):
        for b in range(NB):
            self.dma("X%d" % b, [(self.X[:, b, :], self.x_in[s, b * 128:(b + 1) * 128, :])], [], [self.Xr[b]])

    def store_x(self, s):
        for b in range(NB):
            r = self.res("y%d_%d" % (s, b))
            self.dma("X%d" % b, [(self.y_out[s, b * 128:(b + 1) * 128, :], self.X[:, b, :])], [self.Xr[b]], [r])
            self.out_res.append(r)

    def rms_to_T(self, b, gcol, l, dstT, dstTr, dst_cols):
        X, Xr = self.X, self.Xr
        st = self.ST
        HN, HNr = self.HNs[self.hni % 2], self.HNrs[self.hni % 2]
        self.hni += 1
        self.act(lambda e: e.activation(out=HN[:, :], in_=X[:, b, :], func=AF.Square, accum_out=st[:, 0:1]),
                 [Xr[b]], [HNr, self.STr[0]])
        self.pool(lambda e: e.tensor_scalar(out=st[:, 1:2], in0=st[:, 0:1], scalar1=1.0 / D, scalar2=EPS, op0=ALU.mult, op1=ALU.add),
                  [self.STr[0]], [self.STr[1]])
        self.pool(lambda e: e.tensor_tensor(out=st[:, 2:3], in0=st[:, 1:2], in1=self.NEGH[:, 0:1], op=ALU.pow),
                  [self.STr[1], self.Cr], [self.STr[2]])
        self.act(lambda e: e.activation(out=HN[:, :], in_=X[:, b, :], func=AF.Copy, scale=st[:, 2:3]),
                 [Xr[b], self.STr[2]], [HNr])
        k = self.bank()
        pst = self.PS[k].bitcast(BF16)

        def tr(e):
            ins = None
            for kc in range(8):
                ins = e.transpose(pst[:, kc * 128:(kc + 1) * 128], HN[:, kc * 128:(kc + 1) * 128], self.IDENT[:, :])
            return ins
        self.pe(tr, [HNr, self.Cr], [self.PSr[k]])
        self.dve(lambda e: e.tensor_tensor(out=dstT[:, :, dst_cols], in0=pst[:, :].rearrange("p (k t) -> p k t", k=8),
                                           in1=self.GT[:, l, gcol * 8:(gcol + 1) * 8].unsqueeze(2).to_broadcast([128, 8, 128]), op=ALU.mult),
                 [self.PSr[k], self.Pr], [dstTr])

    def proj(self, c0, ncols, reads_extra=()):
        k = self.bank()
        ps = self.PS[k]
        HT = self.HT

        def fn(e):
            ins = None
            for kc in range(8):
                ins = e.matmul(ps[:, 0:ncols], HT[:, kc, :], self.WIN[:, kc, c0:c0 + ncols], start=(kc == 0), stop=(kc == 7))
            return ins
        self.pe(fn, [self.HTr, self.WINr], [self.PSr[k]])
        return k

    def mixer_phase(self, s, l):
        self.P.phase_switch(self.ffnres, self.mixres)
        self.dma("win", [(self.WIN[:, :, :], self.win_b[l].rearrange("(k p) c -> p k c", p=128))],
                 [self.wres["cv_win%d" % l]], [self.WINr])
        self.dma("wout", [(self.WOUT, self.wout_b[l].rearrange("(k p) c -> p k c", p=128))],
                 [self.wres["cv_wout%d" % l]], [self.WOUTr])
        self.dma("bc", [(self.BC[:, :], self.bc_d[l])], [], [self.BCr])
        self.pool(lambda e: e.memset(self.VAUG[:, :, :, 64:66], 1.0), [], [self.VONESr])
        self.pool(lambda e: e.memset(self.SF[:, :], 0.0), [], [self.SFr])
        self.pool(lambda e: e.memset(self.SB_[:, :], 0.0), [], [self.SBr])
        nb = NB if self.stage >= 3 else 1
        for b in range(nb):
            self.mixer_block(s, l, b)

    def mixer_block(self, s, l, b):
        st = self.ST
        F = self.FAs[b % self.NF[0]]
        Fr = self.FArs[b % self.NF[0]]
        PS, PSr = self.PS, self.PSr
        tapit = (s == 0 and l == 0 and b == 0)
        self.HT, self.HTr = self.HTs[b % 2], self.HTrs[b % 2]
        self.CC, self.CCr = self.CCs[b % self.NCC], self.CCrs[b % self.NCC]
        self.rms_to_T(b, 0, l, self.HT, self.HTr, slice(0, 128))
        if tapit:
            self.tap("hT", self.HT[:, :, :].rearrange("p k t -> p (k t)"), self.HTr, 1024)
        kq = self.proj(OQ, 384)
        kk = self.proj(OK_, 384)
        self.act(lambda e: e.activation(out=F[0][:, 0:384], in_=PS[kq][:, 0:384], func=AF.Copy), [PSr[kq]], [Fr[0]])
        self.act(lambda e: e.activation(out=F[1][:, 0:384], in_=PS[kq][:, 0:384], func=AF.Square), [PSr[kq]], [Fr[1]])
        self.act(lambda e: e.activation(out=F[0][:, 384:768], in_=PS[kk][:, 0:384], func=AF.Copy), [PSr[kk]], [Fr[0]])
        self.act(lambda e: e.activation(out=F[1][:, 384:768], in_=PS[kk][:, 0:384], func=AF.Square), [PSr[kk]], [Fr[1]])
        self.dve(lambda e: e.tensor_reduce(out=st[:, 4:16], in_=F[1][:, :].rearrange("p (h c) -> p h c", c=64), axis=AX.X, op=ALU.add),
                 [Fr[1]], [self.STr[3]])
        self.pool(lambda e: e.tensor_scalar(out=st[:, 16:28], in0=st[:, 4:16], scalar1=1.0 / 64, scalar2=EPS, op0=ALU.mult, op1=ALU.add),
                  [self.STr[3]], [self.STr[4]])
        self.pool(lambda e: e.tensor_tensor(out=st[:, 28:40], in0=st[:, 16:28], in1=self.NEGH[:, 0:12], op=ALU.pow),
                  [self.STr[4], self.Cr], [self.STr[5]])
        self.dve(lambda e: e.tensor_tensor(out=F[1][:, :].rearrange("p (h c) -> p h c", c=64), in0=F[0][:, :].rearrange("p (h c) -> p h c", c=64),
                                           in1=st[:, 28:40].unsqueeze(2).to_broadcast([128, 12, 64]), op=ALU.mult),
                 [Fr[0], self.STr[5]], [Fr[1]])
        self.dve(lambda e: e.tensor_tensor(out=F[0][:, :].rearrange("p (a h c) -> p a h c", a=2, c=64),
                                           in0=F[1][:, :].rearrange("p (a h c) -> p a h c", a=2, c=64),
                                           in1=self.BC[:, 0:128].rearrange("p (a c) -> p a c", a=2).unsqueeze(2).to_broadcast([128, 2, 6, 64]),
                                           op=ALU.mult),
                 [Fr[1], self.BCr], [Fr[0]])
        self.act(lambda e: e.activation(out=self.QKN, in_=F[0][:, :], func=AF.Copy), [Fr[0]], [self.QKNr])
        x = F[0][:, :].rearrange("p (h c) -> p h c", c=64)
        x1, x2 = x[:, :, 0:8], x[:, :, 8:16]
        cosb = self.ROPE[:, 0, b:b + 1, :].to_broadcast([128, 12, 8])
        sinb = self.ROPE[:, 1, b:b + 1, :].to_broadcast([128, 12, 8])
        t = F[2][:, 0:384].rearrange("p (a h c) -> p a h c", a=4, c=8)
        qn = self.QKN.rearrange("p (h c) -> p h c", c=64)
        self.dve(lambda e: e.tensor_tensor(out=t[:, 0], in0=x1, in1=cosb, op=ALU.mult), [Fr[0], self.Cr], [Fr[2]])
        self.dve(lambda e: e.tensor_tensor(out=t[:, 1], in0=x2, in1=sinb, op=ALU.mult), [Fr[0], self.Cr], [Fr[2]])
        self.dve(lambda e: e.tensor_tensor(out=t[:, 2], in0=x2, in1=cosb, op=ALU.mult), [Fr[0], self.Cr], [Fr[2]])
        self.dve(lambda e: e.tensor_tensor(out=t[:, 3], in0=x1, in1=sinb, op=ALU.mult), [Fr[0], self.Cr], [Fr[2]])
        self.dve(lambda e: e.tensor_tensor(out=qn[:, :, 0:8], in0=t[:, 0], in1=t[:, 1], op=ALU.subtract), [Fr[2], self.QKNr], [self.QKNr])
        self.dve(lambda e: e.tensor_tensor(out=qn[:, :, 8:16], in0=t[:, 2], in1=t[:, 3], op=ALU.add), [Fr[2], self.QKNr], [self.QKNr])
        if tapit:
            self.tap("qkn", self.QKN, self.QKNr, 768)
        k = self.bank()
        pst = PS[k].bitcast(BF16)

        def trqk(e):
            ins = None
            for j in range(6):
                ins = e.transpose(pst[:, j * 128:(j + 1) * 128], self.QKN[:, j * 128:(j + 1) * 128], self.IDENT[:, :])
            return ins
        self.pe(trqk, [self.QKNr, self.Cr], [PSr[k]])
        self.act(lambda e: e.activation(out=self.QKT[:, :, b * 128:(b + 1) * 128], in_=pst[:, 0:768].rearrange("p (j t) -> p j t", j=6), func=AF.Copy),
                 [PSr[k]], [self.QKTr[b]])
        kv = self.proj(OV, 384)
        self.act(lambda e: e.activation(out=self.VAUG[:, b, :, 0:64], in_=PS[kv][:, 0:384].rearrange("p (h c) -> p h c", c=64), func=AF.Copy),
                 [PSr[kv]], [self.VAUGr[b]])
        if self.stage < 2:
            return
        self.attention(b, tapit)
        if self.stage < 4:
            return
        self.gla(l, b, tapit)
        if self.stage < 5:
            return
        self.sg(l, b, tapit)
        if self.stage < 6:
            return
        self.outproj(b, tapit)

    def attention(self, b, tapit):
        CC, CCr = self.CC, self.CCr
        PS, PSr = self.PS, self.PSr
        ei = 0
        groups = [list(range(g, min(g + 4, b + 1))) for g in range(0, b + 1, 4)]
        for pr in range(3):
            heads = (2 * pr, 2 * pr + 1)
            for kbs in groups:
                n = len(kbs)
                ks = (self.bank(), self.bank())

                def qk(e, kbs=kbs, ks=ks, pr=pr):
                    ins = None
                    for i, kb in enumerate(kbs):
                        for half in range(2):
                            rows = slice(64 * half, 64 * half + 64)
                            ins = e.matmul(PS[ks[half]][:, i * 128:(i + 1) * 128], self.QKT[rows, 3 + pr, kb * 128:(kb + 1) * 128],
                                           self.QKT[rows, pr, b * 128:(b + 1) * 128], start=True, stop=True)
                    return ins
                self.pe(qk, [self.QKTr[kb] for kb in kbs] + [self.QKTr[b]], [PSr[ks[0]], PSr[ks[1]]])
                j0 = 15 - b + kbs[0]
                for half in range(2):
                    h = heads[half]
                    k = ks[half]
                    ko = 6 + half
                    E, Er = self.E[ei % self.NE], self.Er[ei % self.NE]
                    ei += 1
                    self.act(lambda e, E=E, k=k, n=n: e.activation(out=E[:, 0:n * 128], in_=PS[k][:, 0:n * 128], func=AF.Exp, scale=0.125),
                             [PSr[k]], [Er])
                    mfn = (lambda e, E=E, n=n, j0=j0: e.tensor_tensor(out=E[:, 0:n * 128], in0=E[:, 0:n * 128],
                                                                      in1=self.MASK[:, j0:j0 + n, :].rearrange("p a b -> p (a b)"), op=ALU.mult))
                    if ei % 3 == 0:
                        self.pool(mfn, [Er, self.Cr], [Er])
                    else:
                        self.dve(mfn, [Er, self.Cr], [Er])

                    def pv(e, kbs=kbs, E=E, h=h, ko=ko):
                        ins = None
                        for i, kb in enumerate(kbs):
                            ins = e.matmul(PS[ko][:, 0:65], E[:, i * 128:(i + 1) * 128], self.VAUG[:, kb, h, 0:65],
                                           start=(kb == 0), stop=(kb == b))
                        return ins
                    self.pe(pv, [Er, self.VONESr] + [self.VAUGr[kb] for kb in kbs], [PSr[ko]])
            for half in range(2):
                h = heads[half]
                ko = 6 + half
                sr = self.STr[6 + half]
                sc = self.ST[:, 40 + half:41 + half]
                self.dve(lambda e, sc=sc, ko=ko: e.reciprocal(out=sc, in_=PS[ko][:, 64:65]), [PSr[ko]], [sr])
                self.dve(lambda e, sc=sc, ko=ko, h=h: e.tensor_scalar(out=CC[:, h * 64:(h + 1) * 64], in0=PS[ko][:, 0:64], scalar1=sc, scalar2=None,
                                                                      op0=ALU.mult), [PSr[ko], sr], [CCr[0]])
        if tapit:
            self.tap("att", CC[:, 0:384], CCr[0], 384)

    def gla(self, l, b, tapit):
        CC, CCr = self.CC, self.CCr
        PS, PSr = self.PS, self.PSr
        F, Fr = self.FGs[b % self.NF[1]], self.FGrs[b % self.NF[1]]
        st = self.ST
        kqk = self.proj(OQG, 384)
        self.act(lambda e: e.activation(out=F[0][:, 0:384], in_=PS[kqk][:, 0:384], func=AF.Copy), [PSr[kqk]], [Fr[0]])
        kvg = self.proj(OVG, 384)
        self.act(lambda e: e.activation(out=self.VG[:, :], in_=PS[kvg][:, 0:384], func=AF.Copy), [PSr[kvg]], [self.VGr])
        krg = self.proj(ORG, 384)
        self.act(lambda e: e.activation(out=F[1][:, 0:384], in_=PS[krg][:, 0:384], func=AF.Copy), [PSr[krg]], [Fr[1]])
        self.act(lambda e: e.activation(out=F[1][:, 384:768], in_=PS[krg][:, 0:384], func=AF.Tanh, scale=0.5), [PSr[krg]], [Fr[1]])
        ka = self.bank()

        HT = self.HT

        def fa(e):
            ins = None
            for kc in range(8):
                ins = e.matmul(PS[ka][0:16, 0:128], self.WIN[:, kc, OAG:OAG + 16], HT[:, kc, :], start=(kc == 0), stop=(kc == 7))
            return ins
        self.pe(fa, [self.HTr, self.WINr], [PSr[ka]])
        self.act(lambda e: e.activation(out=self.AGT[0:16, :], in_=PS[ka][0:16, 0:128], func=AF.Copy), [PSr[ka], self.AGTr], [self.AGTr])
        kg = self.bank()

        def fg(e):
            return e.matmul(PS[kg][:, 0:192], self.AGT[0:17, :], self.WA2[:, l, :], start=True, stop=True)
        self.pe(fg, [self.AGTr, self.Pr, self.Cr], [PSr[kg]])
        self.act(lambda e: e.activation(out=F[2][:, 0:192], in_=PS[kg][:, 0:192], func=AF.Exp, scale=-1.0), [PSr[kg]], [Fr[2]])
        self.act(lambda e: e.activation(out=F[2][:, 192:384], in_=F[2][:, 0:192], func=AF.Ln, bias=1.0), [Fr[2]], [Fr[2]])
        lneg = F[2][:, 192:384]
        kc_ = self.bank()

        def fc(e):
            e.matmul(PS[kc_][:, 0:192], self.MISCF[:, 0, :], lneg, start=True, stop=True)
            return e.matmul(PS[kc_][:, 192:384], self.MISCF[:, 1, :], lneg, start=True, stop=True)
        self.pe(fc, [Fr[2], self.Cr], [PSr[kc_]])
        kt = self.bank()

        def ftot(e):
            ins = None
            for h in range(6):
                ins = e.matmul(PS[kt][0:32, h:h + 1], F[2][:, 192 + h * 32:192 + (h + 1) * 32], self.MISCF[:, 2, 0:1], start=True, stop=True)
            return ins
        self.pe(ftot, [Fr[2], self.Cr], [PSr[kt]])
        self.act(lambda e: e.activation(out=self.DEC[:, :], in_=PS[kt][0:32, 0:6], func=AF.Exp, scale=-1.0 / 16), [PSr[kt]], [self.DECr])
        self.act(lambda e: e.activation(out=F[3][:, 0:384], in_=PS[kc_][:, 0:384], func=AF.Exp, scale=-1.0 / 16), [PSr[kc_]], [Fr[3]])
        self.act(lambda e: e.activation(out=F[3][:, 384:576], in_=PS[kc_][:, 0:192], func=AF.Exp, scale=1.0 / 16), [PSr[kc_]], [Fr[3]])
        self.dve(lambda e: e.scalar_tensor_tensor(out=self.GQK[:, 0:192], in0=F[0][:, 0:192], scalar=32.0 ** -0.5, in1=F[3][:, 0:192],
                                                  op0=ALU.mult, op1=ALU.mult), [Fr[0], Fr[3]], [self.GQKr])
        self.dve(lambda e: e.tensor_tensor(out=self.GQK[:, 192:384], in0=F[0][:, 192:384], in1=F[3][:, 384:576], op=ALU.mult),
                 [Fr[0], Fr[3]], [self.GQKr])
        self.dve(lambda e: e.tensor_tensor(out=self.GQK[:, 384:576], in0=F[0][:, 192:384], in1=F[3][:, 192:384], op=ALU.mult),
                 [Fr[0], Fr[3]], [self.GQKr])
        k1 = self.bank()
        k2 = self.bank()
        p1 = PS[k1].bitcast(BF16)
        p2 = PS[k2].bitcast(BF16)

        def ftr(e):
            ins = None
            for h in range(6):
                e.transpose(p1[0:32, h * 128:(h + 1) * 128], self.GQK[:, h * 32:(h + 1) * 32], self.IDENT[:, :])
                ins = e.transpose(p2[0:32, h * 128:(h + 1) * 128], self.GQK[:, 192 + h * 32:192 + (h + 1) * 32], self.IDENT[:, :])
            return ins
        self.pe(ftr, [self.GQKr, self.Cr], [PSr[k1], PSr[k2]])
        self.act(lambda e: e.activation(out=self.GT2[:, 0:6, :], in_=p1[0:32, 0:768].rearrange("p (h t) -> p h t", h=6), func=AF.Copy),
                 [PSr[k1]], [self.GT2r])
        self.act(lambda e: e.activation(out=self.GT2[:, 6:12, :], in_=p2[0:32, 0:768].rearrange("p (h t) -> p h t", h=6), func=AF.Copy),
                 [PSr[k2]], [self.GT2r])
        ka1 = self.bank()
        ka2 = self.bank()

        def fA(e):
            ins = None
            for h in range(6):
                ps = PS[ka1] if h < 4 else PS[ka2]
                c = (h % 4) * 128
                ins = e.matmul(ps[:, c:c + 128], self.GT2[:, 6 + h, :], self.GT2[:, h, :], start=True, stop=True)
            return ins
        self.pe(fA, [self.GT2r], [PSr[ka1], PSr[ka2]])
        self.dve(lambda e: e.tensor_tensor(out=self.AM[:, 0:512].rearrange("p (h t) -> p h t", h=4), in0=PS[ka1][:, 0:512].rearrange("p (h t) -> p h t", h=4),
                                           in1=self.CAUS[:, :].unsqueeze(1).to_broadcast([128, 4, 128]), op=ALU.mult),
                 [PSr[ka1], self.Cr], [self.AMr])
        self.dve(lambda e: e.tensor_tensor(out=self.AM[:, 512:768].rearrange("p (h t) -> p h t", h=2), in0=PS[ka2][:, 0:256].rearrange("p (h t) -> p h t", h=2),
                                           in1=self.CAUS[:, :].unsqueeze(1).to_broadcast([128, 2, 128]), op=ALU.mult),
                 [PSr[ka2], self.Cr], [self.AMr])
        ko = self.bank()

        def fo(e):
            ins = None
            for h in range(6):
                e.matmul(PS[ko][:, h * 64:(h + 1) * 64], self.AM[:, h * 128:(h + 1) * 128], self.VG[:, h * 64:(h + 1) * 64], start=True, stop=False)
                ins = e.matmul(PS[ko][:, h * 64:(h + 1) * 64], self.GT2[:, h, :], self.SB_[:, h * 64:(h + 1) * 64], start=False, stop=True)
            return ins
        self.pe(fo, [self.AMr, self.VGr, self.GT2r, self.SBr], [PSr[ko]])
        ks = self.bank()

        def fkv(e):
            ins = None
            for h in range(6):
                ins = e.matmul(PS[ks][0:32, h * 64:(h + 1) * 64], self.GQK[:, 384 + h * 32:384 + (h + 1) * 32], self.VG[:, h * 64:(h + 1) * 64],
                               start=True, stop=True)
            return ins
        self.pe(fkv, [self.GQKr, self.VGr], [PSr[ks]])
        self.dve(lambda e: e.tensor_tensor(out=self.SF[:, :].rearrange("p (h v) -> p h v", h=6), in0=self.SF[:, :].rearrange("p (h v) -> p h v", h=6),
                                           in1=self.DEC[:, :].unsqueeze(2).to_broadcast([32, 6, 64]), op=ALU.mult),
                 [self.SFr, self.DECr], [self.SFr])
        self.dve(lambda e: e.tensor_tensor(out=self.SF[:, :], in0=PS[ks][0:32, 0:384], in1=self.SF[:, :], op=ALU.add),
                 [PSr[ks], self.SFr], [self.SFr])
        self.dve(lambda e: e.tensor_copy(out=self.SB_[:, :], in_=self.SF[:, :]), [self.SFr], [self.SBr])
        self.act(lambda e: e.activation(out=F[4][:, 0:384], in_=PS[ko][:, 0:384], func=AF.Square), [PSr[ko]], [Fr[4]])
        self.dve(lambda e: e.tensor_reduce(out=st[:, 44:50], in_=F[4][:, 0:384].rearrange("p (h c) -> p h c", c=64), axis=AX.X, op=ALU.add),
                 [Fr[4]], [self.STr[13]])
        self.pool(lambda e: e.tensor_scalar(out=st[:, 50:56], in0=st[:, 44:50], scalar1=1.0 / 64, scalar2=EPS, op0=ALU.mult, op1=ALU.add),
                  [self.STr[13]], [self.STr[14]])
        self.pool(lambda e: e.tensor_tensor(out=st[:, 56:62], in0=st[:, 50:56], in1=self.NEGH[:, 0:6], op=ALU.pow),
                  [self.STr[14], self.Cr], [self.STr[15]])
        self.dve(lambda e: e.scalar_tensor_tensor(out=F[4][:, 384:768], in0=F[1][:, 384:768], scalar=1.0, in1=F[1][:, 0:384], op0=ALU.add, op1=ALU.mult),
                 [Fr[1]], [Fr[4]])
        self.dve(lambda e: e.tensor_tensor(out=F[4][:, 0:384].rearrange("p (h c) -> p h c", c=64), in0=PS[ko][:, 0:384].rearrange("p (h c) -> p h c", c=64),
                                           in1=st[:, 56:62].unsqueeze(2).to_broadcast([128, 6, 64]), op=ALU.mult),
                 [PSr[ko], self.STr[15], Fr[4]], [Fr[4]])
        self.dve(lambda e: e.tensor_tensor(out=F[4][:, 0:384], in0=F[4][:, 0:384], in1=F[4][:, 384:768], op=ALU.mult), [Fr[4]], [Fr[4]])
        self.dve(lambda e: e.scalar_tensor_tensor(out=CC[:, 384:768].rearrange("p (h c) -> p h c", c=64), in0=F[4][:, 0:384].rearrange("p (h c) -> p h c", c=64),
                                                  scalar=0.5, in1=self.BC[:, 128:192].unsqueeze(1).to_broadcast([128, 6, 64]),
                                                  op0=ALU.mult, op1=ALU.mult), [Fr[4], self.BCr], [CCr[1]])
        if tapit:
            self.tap("gla", CC[:, 384:768], CCr[1], 384)

    def sg(self, l, b, tapit):
        CC, CCr = self.CC, self.CCr
        PS, PSr = self.PS, self.PSr
        S0, S1 = self.FSs[b % self.NF[2]]
        S0r, S1r = self.FSrs[b % self.NF[2]]
        st = self.ST
        kz = self.proj(OZS, 512)
        z = PS[kz][:, 0:512]
        C0 = 0.7978845608028654
        self.act(lambda e: e.activation(out=S1[:, :], in_=z, func=AF.Copy), [PSr[kz]], [S1r])
        self.act(lambda e: e.activation(out=S0[:, :], in_=z, func=AF.Square), [PSr[kz]], [S0r])
        self.dve(lambda e: e.tensor_scalar(out=S0[:, :], in0=S0[:, :], scalar1=0.044715, scalar2=1.0, op0=ALU.mult, op1=ALU.add),
                 [S0r], [S0r])
        self.dve(lambda e: e.tensor_tensor(out=S0[:, :], in0=S1[:, :], in1=S0[:, :], op=ALU.mult), [S1r, S0r], [S0r])
        self.act(lambda e: e.activation(out=S0[:, :], in_=S0[:, :], func=AF.Tanh, scale=C0), [S0r], [S0r])
        self.dve(lambda e: e.scalar_tensor_tensor(out=S1[:, :], in0=S0[:, :], scalar=1.0, in1=S1[:, :], op0=ALU.add, op1=ALU.mult),
                 [S0r, S1r], [S1r])
        u2 = S1[:, 0:256]
        v2 = S1[:, 256:512]
        self.dve(lambda e: e.tensor_reduce(out=st[:, 62:63], in_=v2, axis=AX.X, op=ALU.add), [S1r], [self.STr[8]])
        self.pool(lambda e: e.tensor_scalar(out=st[:, 63:64], in0=st[:, 62:63], scalar1=-1.0 / 256, scalar2=None, op0=ALU.mult),
                  [self.STr[8]], [self.STr[9]])
        self.act(lambda e: e.activation(out=S0[:, 0:256], in_=v2, func=AF.Identity, bias=st[:, 63:64]), [S1r, self.STr[9], S0r], [S0r])
        self.act(lambda e: e.activation(out=S0[:, 256:512], in_=S0[:, 0:256], func=AF.Square, accum_out=st[:, 3:4]), [S0r], [S0r, self.STr[10]])
        self.pool(lambda e: e.tensor_scalar(out=st[:, 42:43], in0=st[:, 3:4], scalar1=1.0 / 256, scalar2=4.0 * EPS, op0=ALU.mult, op1=ALU.add),
                  [self.STr[10]], [self.STr[11]])
        self.pool(lambda e: e.tensor_tensor(out=st[:, 43:44], in0=st[:, 42:43], in1=self.NEGH[:, 0:1], op=ALU.pow),
                  [self.STr[11], self.Cr], [self.STr[12]])
        self.dve(lambda e: e.scalar_tensor_tensor(out=S0[:, 256:512], in0=S0[:, 0:256], scalar=st[:, 43:44], in1=self.BC[:, 192:448],
                                                  op0=ALU.mult, op1=ALU.mult), [S0r, self.STr[12], self.BCr], [S0r])
        self.dve(lambda e: e.tensor_tensor(out=self.VLN[:, :], in0=S0[:, 256:512], in1=self.BC[:, 448:704], op=ALU.add),
                 [S0r, self.BCr], [self.VLNr])
        ks = self.bank()

        def fs(e):
            ins = None
            for g in range(4):
                ins = e.matmul(PS[ks][:, g * 64:(g + 1) * 64], self.SGW[:, l, g, :], self.VLN[:, g * 64:(g + 1) * 64], start=True, stop=True)
            return ins
        self.pe(fs, [self.VLNr, self.Pr], [PSr[ks]])
        self.dve(lambda e: e.tensor_tensor(out=S0[:, 0:256].rearrange("p (g c) -> p g c", g=4), in0=PS[ks][:, 0:256].rearrange("p (g c) -> p g c", g=4),
                                           in1=self.SGB[:, l, :].unsqueeze(2).to_broadcast([128, 4, 64]), op=ALU.add),
                 [PSr[ks], self.Pr, S0r], [S0r])
        self.dve(lambda e: e.scalar_tensor_tensor(out=CC[:, 768:1024], in0=u2, scalar=0.5, in1=S0[:, 0:256], op0=ALU.mult, op1=ALU.mult),
                 [S1r, S0r], [CCr[2]])
        if tapit:
            self.tap("sg", CC[:, 768:1024], CCr[2], 256)

    def outproj(self, b, tapit):
        CC, CCr = self.CC, self.CCr
        PS, PSr = self.PS, self.PSr
        k = self.bank()
        pst = PS[k].bitcast(BF16)

        def tr(e):
            ins = None
            for kc in range(8):
                ins = e.transpose(pst[:, kc * 128:(kc + 1) * 128], CC[:, kc * 128:(kc + 1) * 128], self.IDENT[:, :])
            return ins
        self.pe(tr, CCr + [self.Cr], [PSr[k]])
        self.act(lambda e: e.activation(out=self.CT, in_=pst[:, :].rearrange("p (k t) -> p k t", k=8), func=AF.Copy), [PSr[k]], [self.CTr])
        for n in range(2):
            k2 = self.bank()

            def mm(e, k2=k2, n=n):
                ins = None
                for kc in range(8):
                    ins = e.matmul(PS[k2][:, :], self.CT[:, kc, :], self.WOUT[:, kc, n * 512:(n + 1) * 512], start=(kc == 0), stop=(kc == 7))
                return ins
            self.pe(mm, [self.CTr, self.WOUTr], [PSr[k2]])
            self.dve(lambda e, k2=k2, n=n: e.tensor_tensor(out=self.X[:, b, n * 512:(n + 1) * 512], in0=PS[k2][:, :], in1=self.X[:, b, n * 512:(n + 1) * 512], op=ALU.add),
                     [PSr[k2], self.Xr[b]], [self.Xr[b]])

    def ffn_phase(self, s, l):
        PS, PSr = self.PS, self.PSr
        TT = self.TT
        nblk = TT // 128
        w1 = self.wff1_b[l].rearrange("(k p) c -> p k c", p=128)
        w2 = self.wff2_b[l].rearrange("(k p) c -> p k c", p=128)
        ntile = S // TT
        self.P.phase_switch(self.mixres, self.ffnres)
        items = [(t, j) for t in range(ntile) for j in range(8)]

        def norms(t):
            H2T, H2Tr = self.H2Ts[t % 2], self.H2Trs[t % 2]
            for i in range(nblk):
                self.rms_to_T(t * nblk + i, 1, l, H2T, H2Tr, slice(i * 128, (i + 1) * 128))

        def ff1(idx):
            t, j = items[idx]
            sl = idx % 2
            H2T, H2Tr = self.H2Ts[t % 2], self.H2Trs[t % 2]
            self.dma("w1_%d" % sl, [(self.W1[sl], w1[:, :, j * 512:(j + 1) * 512])], [self.wres["cv_ff1%d" % l]], [self.W1r[sl]])
            H1, H1r = self.H1T[sl], self.H1Tr[sl]
            for fc in range(4):
                for c0 in range(0, TT, 512):
                    k = self.bank()

                    def f1(e, k=k, fc=fc, sl=sl, c0=c0, H2T=H2T):
                        ins = None
                        for kc in range(8):
                            ins = e.matmul(PS[k][:, :], self.W1[sl][:, kc, fc * 128:(fc + 1) * 128], H2T[:, kc, c0:c0 + 512],
                                           start=(kc == 0), stop=(kc == 7))
                        return ins
                    self.pe(f1, [self.W1r[sl], H2Tr], [PSr[k]])
                    ri = (fc + c0 // 512) % 2
                    self.act(lambda e, k=k, ri=ri: e.activation(out=self.RELU[ri][:, 0:512], in_=PS[k][:, :], func=AF.Relu), [PSr[k]], [self.RELUr[ri]])
                    self.pool(lambda e, ri=ri, fc=fc, H1=H1, c0=c0: e.tensor_tensor(out=H1[:, fc, c0:c0 + 512], in0=self.RELU[ri][:, 0:512], in1=self.RELU[ri][:, 0:512], op=ALU.mult),
                              [self.RELUr[ri]], [H1r])

        def ff2(idx):
            t, j = items[idx]
            sl = idx % 2
            self.dma("w2_%d" % sl, [(self.W2[sl], w2[:, j * 4:(j + 1) * 4, :])], [self.wres["cv_ff2%d" % l]], [self.W2r[sl]])
            H1, H1r = self.H1T[sl], self.H1Tr[sl]
            for i in range(nblk):
                b = t * nblk + i
                for n in range(2):
                    k = self.bank()

                    def f2(e, k=k, i=i, n=n, sl=sl, H1=H1):
                        ins = None
                        for fc in range(4):
                            ins = e.matmul(PS[k][:, :], H1[:, fc, i * 128:(i + 1) * 128], self.W2[sl][:, fc, n * 512:(n + 1) * 512],
                                           start=(fc == 0), stop=(fc == 3))
                        return ins
                    self.pe(f2, [H1r, self.W2r[sl]], [PSr[k]])
                    self.dve(lambda e, k=k, b=b, n=n: e.tensor_tensor(out=self.X[:, b, n * 512:(n + 1) * 512], in0=PS[k][:, :],
                                                                       in1=self.X[:, b, n * 512:(n + 1) * 512], op=ALU.add),
                             [PSr[k], self.Xr[b]], [self.Xr[b]])

        norms(0)
        for idx in range(len(items)):
            t, j = items[idx]
            if j == 4 and t + 1 < ntile:
                norms(t + 1)
            ff1(idx)
            if idx > 0:
                ff2(idx - 1)
        ff2(len(items) - 1)


def _constants():
    k = np.arange(128)[:, None]
    q = np.arange(128)[None, :]
    mask = np.zeros((128, 16, 128), np.float32)
    for j in range(16):
        delta = 15 - j
        dist = 128 * delta + q - k
        m1 = (dist >= 0) & (dist <= 128)
        m4 = (dist % 4 == 0) & (dist >= 0) & (dist <= 512)
        m16 = (dist % 16 == 0) & (dist >= 0) & (dist <= 2048)
        mask[:, j, :] = m1.astype(np.float32) + m4.astype(np.float32) + m16.astype(np.float32)
    ident = np.eye(128, dtype=np.float32)
    causal = (k <= q).astype(np.float32)
    triu = (k > q).astype(np.float32)
    ones = np.ones((128, 128), np.float32)
    misc = np.concatenate([ident, causal, triu, ones], axis=1)
    inv = (500000.0 ** (-np.arange(0, 16, 2, dtype=np.float32) / 16.0)).astype(np.float32)
    pos = (np.arange(16)[None, :] * 128 + np.arange(128)[:, None]).astype(np.float32)
    ang = pos[:, :, None] * inv[None, None, :]
    rope = np.concatenate([np.cos(ang).reshape(128, 128), np.sin(ang).reshape(128, 128)], axis=1).astype(np.float32)
    return mask.reshape(128, 2048), misc, rope


def _host_inputs(inp):
    f = lambda a: np.ascontiguousarray(np.asarray(a, dtype=np.float32))
    mask, misc, rope = _constants()
    n1, n2 = f(inp["norm1_g"]), f(inp["norm2_g"])
    gT = np.concatenate([n1.reshape(DEPTH, 8, 128).transpose(0, 2, 1), n2.reshape(DEPTH, 8, 128).transpose(0, 2, 1)], axis=2)
    row = np.concatenate([f(inp["q_norm_g"]), f(inp["k_norm_g"]), f(inp["gla_norm_g"]), f(inp["sg_ln_g"]), f(inp["sg_ln_b"])], axis=1)
    bc = np.broadcast_to(row[:, None, :], (DEPTH, 128, 704))
    common = {
        "w_in": f(inp["w_in"]), "w_out": f(inp["w_out"]), "w_ff1": f(inp["w_ff1"]), "w_ff2": f(inp["w_ff2"]),
        "gT": f(gT), "bc": f(bc), "wa2": f(inp["gla_w_a2"]), "ba": f(inp["gla_b_a"]).reshape(DEPTH, 1, 192),
        "sgwT": f(np.transpose(f(inp["sg_w"]), (0, 3, 1, 2))), "sgbT": f(np.transpose(f(inp["sg_b"]), (0, 2, 1))),
        "c_mask": mask, "c_misc": misc, "c_rope": rope,
    }
    return common


_NC_CACHE = {}


def kernel(**inputs):
    x = np.ascontiguousarray(np.asarray(inputs["x"], dtype=np.float32))
    common = _host_inputs(inputs)
    if "nc" not in _NC_CACHE:
        _NC_CACHE["nc"] = Builder().build()
    nc = _NC_CACHE["nc"]
    in_maps = []
    for c in range(NCORES):
        m = dict(common)
        m["x"] = x[c * SEQ_PER_CORE:(c + 1) * SEQ_PER_CORE]
        in_maps.append(m)
    res = run_bass_kernel_spmd(nc, in_maps, core_ids=list(range(NCORES)))
    out = np.concatenate([np.asarray(r["y"]) for r in res.results], axis=0)
    return out.astype(np.float32)
```

```python
import numpy as np
from contextlib import ExitStack
import concourse.bass as bass
import concourse.mybir as mybir
from concourse.bass_utils import run_bass_kernel_spmd

F32 = mybir.dt.float32
BF16 = mybir.dt.bfloat16
AF = mybir.ActivationFunctionType
ALU = mybir.AluOpType
AX = mybir.AxisListType

D = 1024
S = 2048
NB = 16
DEPTH = 2
NCORES = 8
SEQ_PER_CORE = 4
IN_DIM = 2832
DFF = 4096
EPS = 1e-6
OQ, OK_, OV, OQG, OKG, OVG, ORG, OAG, OZS = 0, 384, 768, 1152, 1344, 1536, 1920, 2304, 2320
HS = 66


class Res:
    __slots__ = ("name", "w", "rs", "extra")

    def __init__(self, name):
        self.name = name
        self.w = None
        self.rs = []
        self.extra = None


class Op:
    __slots__ = ("eng", "fn", "deps", "odeps", "sig", "cnt", "dkey", "dval", "isdma", "ndma", "idx", "dur", "nbytes",
                 "succ", "nleft", "rt", "fin")


SEM_LAT = 150.0
DMA_BW = 270.0
DMA_LAT = 2000.0


class Prog:
    ENGS = ("pe", "act", "dve", "pool", "sp")

    use_bl = True

    def __init__(self):
        self.all = []
        self.nops = 0
        self.dma_keys = set()

    def add(self, eng, fn, reads=(), writes=(), dkey=None, ndma=1, dur=300.0, nbytes=0):
        o = Op()
        o.eng = eng
        o.fn = fn
        o.sig = False
        o.cnt = 0
        o.isdma = dkey is not None
        o.dkey = dkey
        o.ndma = ndma
        o.dur = dur
        o.nbytes = nbytes
        o.idx = len(self.all)
        deps = {}

        def need(p, kind):
            if p is None or p is o:
                return
            ent = deps.get(id(p))
            if ent is None:
                deps[id(p)] = (p, {kind})
            else:
                ent[1].add(kind)

        for r in reads:
            need(r.w, "RAW")
        for w in writes:
            need(w.w, "WAW")
            for rd in w.rs:
                need(rd, "WAR")
            if w.extra:
                for p in w.extra:
                    need(p, "WAR")
                w.extra = None
        sync, order = [], []
        for p, kinds in deps.values():
            if p.isdma:
                sync.append(p)
            elif p.eng == eng:
                if eng != "pe" and ("RAW" in kinds or eng == "pool"):
                    sync.append(p)
                    p.sig = True
                else:
                    order.append(p)
            else:
                sync.append(p)
                p.sig = True
        o.deps = sync
        o.odeps = order
        for r in reads:
            r.rs.append(o)
        for w in writes:
            w.w = o
            w.rs = []
        if o.isdma:
            self.dma_keys.add(dkey)
        self.all.append(o)
        self.nops += 1
        return o

    def phase_switch(self, from_res, to_res):
        seen = {}
        for r in from_res:
            if r.w is not None:
                seen[id(r.w)] = r.w
            for p in r.rs:
                seen[id(p)] = p
        L = list(seen.values())
        for r in to_res:
            r.extra = (r.extra or []) + L

    def schedule(self, enable=True):
        ops = self.all
        self.ops = {e: [] for e in self.ENGS}
        if not enable:
            for o in ops:
                self.ops[o.eng].append(o)
            self.makespan = 0.0
            return
        for o in ops:
            o.succ = []
            o.rt = 0.0
            o.fin = None
        for o in ops:
            n = 0
            for p in o.deps:
                p.succ.append((o, True))
                n += 1
            for p in o.odeps:
                p.succ.append((o, False))
                n += 1
            o.nleft = n
        bl = [0.0] * len(ops)
        for o in reversed(ops):
            m = 0.0
            for (q, issync) in o.succ:
                v = bl[q.idx] + (SEM_LAT if issync else 0.0)
                if v > m:
                    m = v
            d = o.dur if not o.isdma else (o.nbytes / DMA_BW + DMA_LAT)
            bl[o.idx] = m + d
        ready = {e: [] for e in self.ENGS}
        for o in ops:
            if o.nleft == 0:
                ready[o.eng].append(o)
        free = {e: 0.0 for e in self.ENGS}
        dma_free = 0.0
        left = len(ops)
        while left:
            best = None
            bkey = None
            for e in self.ENGS:
                fe = free[e]
                for o in ready[e]:
                    st = o.rt if o.rt > fe else fe
                    key = (st, -bl[o.idx] if self.use_bl else o.idx, o.idx)
                    if bkey is None or key < bkey:
                        bkey = key
                        best = o
            o = best
            st = bkey[0]
            ready[o.eng].remove(o)
            if o.isdma:
                free[o.eng] = st + 60.0 * o.ndma
                xs = max(st + 60.0, dma_free)
                dma_free = xs + o.nbytes / DMA_BW
                o.fin = dma_free + DMA_LAT
            else:
                o.fin = st + o.dur
                free[o.eng] = o.fin
            self.ops[o.eng].append(o)
            for (q, issync) in o.succ:
                t = o.fin + (SEM_LAT if (issync and (q.eng != o.eng or o.isdma)) else (60.0 if issync else 0.0))
                if t > q.rt:
                    q.rt = t
                q.nleft -= 1
                if q.nleft == 0:
                    ready[q.eng].append(q)
            left -= 1
        self.makespan = max(o.fin for o in ops)

    def emit(self, nc, stack):
        esem = {}
        for e in ("pe", "act", "dve", "pool"):
            esem[e] = stack.enter_context(nc.semaphore("s_" + e))
        dsem = {}
        for k in sorted(self.dma_keys):
            dsem[k] = stack.enter_context(nc.semaphore("d_" + str(k)))
        tot = {}
        for e in self.ENGS:
            for o in self.ops[e]:
                if o.isdma:
                    tot[o.dkey] = tot.get(o.dkey, 0) + 16 * o.ndma
                    o.dval = tot[o.dkey]
        for e in ("pe", "act", "dve", "pool"):
            c = 0
            for o in self.ops[e]:
                if o.sig and not o.isdma:
                    c += 1
                    o.cnt = c
        self.sig_counts = {e: sum(1 for o in self.ops[e] if o.sig and not o.isdma) for e in esem}

        def run(engname, e):
            known = {}
            for o in self.ops[engname]:
                waits = {}
                for p in o.deps:
                    if p.isdma:
                        s, v = dsem[p.dkey], p.dval
                    else:
                        s, v = esem[p.eng], p.cnt
                    if known.get(s, 0) >= v:
                        continue
                    if waits.get(s, (None, 0))[1] < v:
                        waits[s] = (s, v)
                for s, v in waits.values():
                    e.wait_ge(s, v)
                    known[s] = v
                if o.isdma:
                    o.fn(e, dsem[o.dkey])
                else:
                    ins = o.fn(e)
                    if o.sig:
                        ins.then_inc(esem[engname], 1)

        block = stack.enter_context(nc.Block())

        @block.tensor
        def _(e):
            run("pe", e)

        @block.scalar
        def _(e):
            run("act", e)

        @block.vector
        def _(e):
            run("dve", e)

        @block.gpsimd
        def _(e):
            run("pool", e)

        @block.sync
        def _(e):
            run("sp", e)


class _Dummy:
    def then_inc(self, *a, **k):
        return self


class _Rec:
    def __init__(self):
        self.calls = []

    def __getattr__(self, name):
        def f(*a, **k):
            self.calls.append((name, a, k))
            return _Dummy()
        return f


class Builder:
    def __init__(self, nseq=SEQ_PER_CORE, nlayers=DEPTH, stage=99, dbg_cols=0, tt=512, sched=True):
        self.sched = sched
        self.NE = 6
        self.NCC = 1
        self.NF = (1, 1, 1)
        self.nseq = nseq
        self.nlayers = nlayers
        self.stage = stage
        self.dbg_cols = dbg_cols
        self.dbg_off = 0
        self.taps = {}
        self.TT = tt
        self.nc = bass.Bass("TRN2", target_bir_lowering=False, dynamic_dma_scratch_size=256)
        self.P = Prog()
        self.stack = ExitStack()
        self.uid = 0
        self.rr = 0
        self.tapi = 0

    def sb(self, name, shape, dt):
        return self.stack.enter_context(self.nc.sbuf_tensor(name, list(shape), dt))

    def dram(self, name, shape, dt, kind):
        return self.nc.dram_tensor(name, list(shape), dt, kind=kind).ap()

    def res(self, name):
        return Res(name)

    def est(self, eng, fn, isdma=False):
        rec = _Rec()
        if isdma:
            fn(rec, None)
        else:
            fn(rec)
        dur = 0.0
        nbytes = 0
        for (name, a, k) in rec.calls:
            if name == "matmul":
                rhs = a[2] if len(a) > 2 else k["rhs"]
                n = int(np.prod(rhs.shape[1:]))
                if rhs.dtype == F32:
                    d = max(60.0, 2.0 * n)
                else:
                    d = max(105.0, 0.52 * n + 30.0)
                dur += d
            elif name == "transpose":
                dur += 105.0
            elif name == "activation":
                n = int(np.prod(k["in_"].shape[1:]))
                dur += 190.0 + 0.78 * n + (100.0 if k.get("accum_out") is not None else 0.0)
            elif name == "dma_start":
                o = k["out"]
                nbytes += int(np.prod(o.shape)) * mybir.dt.size(o.dtype)
            elif name == "nop":
                dur += 50.0
            else:
                o = k.get("out", a[0] if a else None)
                n = int(np.prod(o.shape[1:])) if o is not None else 64
                if eng == "pool":
                    dur += 1000.0 if k.get("op") == ALU.pow else 150.0 + 1.8 * n
                else:
                    dur += 150.0 + 1.0 * n
        return dur, nbytes

    def pe(self, fn, reads, writes):
        return self.P.add("pe", fn, reads, writes, dur=self.est("pe", fn)[0])

    def act(self, fn, reads, writes):
        return self.P.add("act", fn, reads, writes, dur=self.est("act", fn)[0])

    def dve(self, fn, reads, writes):
        return self.P.add("dve", fn, reads, writes, dur=self.est("dve", fn)[0])

    def pool(self, fn, reads, writes):
        return self.P.add("pool", fn, reads, writes, dur=self.est("pool", fn)[0])

    def dma(self, key, pairs, reads, writes):
        def fn(e, sem, pairs=pairs):
            for (o, i) in pairs:
                e.dma_start(out=o, in_=i).then_inc(sem, 16)
        return self.P.add("sp", fn, reads, writes, dkey=key, ndma=len(pairs), nbytes=self.est("sp", fn, True)[1])

    def bank(self):
        k = self.rr % 6
        self.rr += 1
        return k

    def tap(self, name, ap, res, ncols, parts=128):
        if self.dbg_cols == 0:
            return
        off = self.dbg_off
        self.dbg_off += ncols
        assert self.dbg_off <= self.dbg_cols, self.dbg_off
        self.taps[name] = (off, ncols, parts)
        for c0 in range(0, ncols, 512):
            n = min(512, ncols - c0)
            i = self.tapi % 2
            self.tapi += 1
            st, r = self.RELU[i], self.RELUr[i]
            self.pool(lambda e, st=st, c0=c0, n=n: e.tensor_copy(out=st[:, 0:n], in_=ap[:, c0:c0 + n]), [res], [r])
            self.dma("tap%d" % i, [(self.dbg[:, off + c0:off + c0 + n], st[:, 0:n])], [r], [self.res("dbgd_" + name)])

    def build(self):
        nc = self.nc
        nseq, L = self.nseq, self.nlayers
        self.x_in = self.dram("x", [nseq, S, D], F32, "ExternalInput")
        self.y_out = self.dram("y", [nseq, S, D], F32, "ExternalOutput")
        self.w_in_f = self.dram("w_in", [DEPTH, D, IN_DIM], F32, "ExternalInput")
        self.w_out_f = self.dram("w_out", [DEPTH, D, D], F32, "ExternalInput")
        self.w_ff1_f = self.dram("w_ff1", [DEPTH, D, DFF], F32, "ExternalInput")
        self.w_ff2_f = self.dram("w_ff2", [DEPTH, DFF, D], F32, "ExternalInput")
        self.gT_d = self.dram("gT", [DEPTH, 128, 16], F32, "ExternalInput")
        self.bc_d = self.dram("bc", [DEPTH, 128, 704], F32, "ExternalInput")
        self.wa2_d = self.dram("wa2", [DEPTH, 16, 192], F32, "ExternalInput")
        self.ba_d = self.dram("ba", [DEPTH, 1, 192], F32, "ExternalInput")
        self.sgw_d = self.dram("sgwT", [DEPTH, 128, 4, 128], F32, "ExternalInput")
        self.sgb_d = self.dram("sgbT", [DEPTH, 128, 4], F32, "ExternalInput")
        self.cmask_d = self.dram("c_mask", [128, 16 * 128], F32, "ExternalInput")
        self.cmisc_d = self.dram("c_misc", [128, 4 * 128], F32, "ExternalInput")
        self.crope_d = self.dram("c_rope", [128, 16 * 16], F32, "ExternalInput")
        if self.dbg_cols:
            self.dbg = self.dram("dbg", [128, self.dbg_cols], F32, "ExternalOutput")
        self.win_b = self.dram("win_b", [DEPTH, D, IN_DIM], BF16, "Internal")
        self.wout_b = self.dram("wout_b", [DEPTH, D, D], BF16, "Internal")
        self.wff1_b = self.dram("wff1_b", [DEPTH, D, DFF], BF16, "Internal")
        self.wff2_b = self.dram("wff2_b", [DEPTH, DFF, D], BF16, "Internal")

        self.X = self.sb("X", [128, NB, D], F32 if not getattr(self, "simshrink", False) else BF16)
        self.Xr = [self.res("X%d" % b) for b in range(NB)]
        self.WIN = self.sb("WIN", [128, 8, IN_DIM], BF16)
        self.WINr = self.res("WIN")
        REG_ELEMS = 28672
        self.REG = self.sb("REG", [128, REG_ELEMS], BF16)
        R = self.REG
        o = 0
        self.KT = R[:, o:o + 3 * S].rearrange("p (j t) -> p j t", j=3); o += 3 * S
        self.QT = [R[:, o + i * 384:o + (i + 1) * 384].rearrange("p (j t) -> p j t", j=3) for i in range(2)]; o += 768
        NE = self.NE
        self.E = [R[:, o + i * 512:o + (i + 1) * 512] for i in range(NE)]; o += NE * 512
        self.VAUG = R[:, o:o + NB * 6 * HS].rearrange("p (b h c) -> p b h c", b=NB, h=6); o += NB * 6 * HS
        self.WOUT = R[:, o:o + 8 * D].rearrange("p (k c) -> p k c", k=8); o += 8 * D
        self.CT = R[:, o:o + 8 * 128].rearrange("p (k t) -> p k t", k=8); o += 8 * 128
        self.QKN = R[:, o:o + 768]; o += 768
        assert o <= REG_ELEMS, o
        TT = self.TT
        o = 0
        self.H2Ts = [R[:, o + i * 8 * TT:o + (i + 1) * 8 * TT].rearrange("p (k t) -> p k t", k=8) for i in range(2)]; o += 2 * 8 * TT
        self.H1T = [R[:, o + i * 4 * TT:o + (i + 1) * 4 * TT].rearrange("p (k t) -> p k t", k=4) for i in range(2)]; o += 2 * 4 * TT
        self.W1 = [R[:, o + i * 4096:o + (i + 1) * 4096].rearrange("p (k c) -> p k c", k=8) for i in range(2)]; o += 2 * 4096
        self.W2 = [R[:, o + i * 4096:o + (i + 1) * 4096].rearrange("p (k c) -> p k c", k=4) for i in range(2)]; o += 2 * 4096
        assert o <= REG_ELEMS, o
        self.QKTr = [self.res("KT%d" % b) for b in range(NB)]
        self.QTr = [self.res("QT0"), self.res("QT1")]
        self.Er = [self.res("E%d" % i) for i in range(self.NE)]
        self.VAUGr = [self.res("VAUG%d" % b) for b in range(NB)]
        self.VONESr = self.res("VONES")
        self.WOUTr = self.res("WOUT")
        self.CTr = self.res("CT")
        self.QKNr = self.res("QKN")
        self.H2Trs = [self.res("H2T0"), self.res("H2T1")]
        self.H1Tr = [self.res("H1T0"), self.res("H1T1")]
        self.W1r = [self.res("W1_0"), self.res("W1_1")]
        self.W2r = [self.res("W2_0"), self.res("W2_1")]
        self.mixres = self.QKTr + self.QTr + self.Er + self.VAUGr + [self.VONESr, self.WOUTr, self.CTr, self.QKNr]
        self.ffnres = self.H2Trs + self.H1Tr + self.W1r + self.W2r

        self.MASK = self.sb("MASK", [128, 16, 128], BF16)
        self.IDENT = self.sb("IDENT", [128, 128], BF16)
        self.CAUS = self.sb("CAUS", [128, 128], BF16)
        self.MISCF = self.sb("MISCF", [128, 3, 128], F32)
        self.ROPE = self.sb("ROPE", [128, 2, NB, 8], F32)
        self.Cr = self.res("consts")
        self.GT = self.sb("GT", [128, DEPTH, 16], F32)
        self.BC = self.sb("BC", [128, 704], F32); self.BCr = self.res("BC")
        self.WA2 = self.sb("WA2", [17, DEPTH, 192], F32)
        self.SGW = self.sb("SGW", [128, DEPTH, 4, 128], BF16)
        self.SGB = self.sb("SGB", [128, DEPTH, 4], F32)
        self.NEGH = self.sb("NEGH", [128, 16], F32)
        self.Pr = self.res("params")

        NF = self.NF
        self.FAs = [[self.sb("FA%d_%d" % (i, j), [128, n], F32) for i, n in enumerate((768, 768, 384))] for j in range(NF[0])]
        self.FArs = [[self.res("FA%d_%d" % (i, j)) for i in range(3)] for j in range(NF[0])]
        self.FGs = [[self.sb("FG%d_%d" % (i, j), [128, n], F32) for i, n in enumerate((384, 768, 384, 576, 768))] for j in range(NF[1])]
        self.FGrs = [[self.res("FG%d_%d" % (i, j)) for i in range(5)] for j in range(NF[1])]
        self.FSs = [[self.sb("FS%d_%d" % (i, j), [128, 512], F32) for i in range(2)] for j in range(NF[2])]
        self.FSrs = [[self.res("FS%d_%d" % (i, j)) for i in range(2)] for j in range(NF[2])]
        self.FA, self.FAr = self.FAs[0], self.FArs[0]
        self.HNs = [self.sb("HN%d" % i, [128, D], BF16) for i in range(2)]; self.HNrs = [self.res("HN0"), self.res("HN1")]
        self.HTs = [self.sb("HT%d" % i, [128, 8, 128], BF16) for i in range(2)]; self.HTrs = [self.res("HT0"), self.res("HT1")]
        self.hni = 0
        NE = self.NE
        self.CCs = [self.sb("CC%d" % i, [128, D], BF16) for i in range(self.NCC)]
        self.CCrs = [[self.res("CCatt%d" % i), self.res("CCgla%d" % i), self.res("CCsg%d" % i)] for i in range(self.NCC)]
        self.ST = self.sb("ST", [128, 64], F32); self.STr = [self.res("ST%d" % i) for i in range(16)]
        self.AGT = self.sb("AGT", [32, 128], F32); self.AGTr = self.res("AGT")
        self.GQK = self.sb("GQK", [128, 576], BF16); self.GQKr = self.res("GQK")
        self.GT2 = self.sb("GT2", [32, 12, 128], BF16); self.GT2r = self.res("GT2")
        self.VG = self.sb("VG", [128, 384], BF16); self.VGr = self.res("VG")
        self.AM = self.sb("AM", [128, 768], BF16); self.AMr = self.res("AM")
        self.SF = self.sb("SF", [32, 384], F32); self.SFr = self.res("SF")
        self.SB_ = self.sb("SBF", [32, 384], BF16); self.SBr = self.res("SBF")
        self.DEC = self.sb("DEC", [32, 6], F32); self.DECr = self.res("DEC")
        self.VLN = self.sb("VLN", [128, 256], BF16); self.VLNr = self.res("VLN")
        self.RELU = [self.FA[0], self.FA[1]]
        self.RELUr = [self.FAr[0], self.FAr[1]]

        self.PS = [self.stack.enter_context(nc.psum_tensor("ps%d" % i, [128, 512], F32)) for i in range(8)]
        self.PSr = [self.res("ps%d" % i) for i in range(8)]

        self.setup()
        if self.stage >= 1:
            self.convert_weights()
        self.marks = [("setup+convert", len(self.P.all))]
        for s in range(nseq):
            self.load_x(s)
            for l in range(L):
                self.mixer_phase(s, l)
                self.marks.append(("mix s%d l%d" % (s, l), len(self.P.all)))
                if self.stage >= 8:
                    self.ffn_phase(s, l)
                    self.marks.append(("ffn s%d l%d" % (s, l), len(self.P.all)))
            self.store_x(s)
        self.P.add("sp", lambda e: e.nop(), self.out_res, [], dur=50.0)
        self.P.schedule(self.sched)
        if getattr(self, "simshrink", False):
            self.stack.close()
            return None
        self.P.emit(nc, self.stack)
        self.stack.close()
        return nc

    def setup(self):
        stg = self.X
        sr = self.Xr
        self.out_res = []
        self.dma("c0", [(stg[:, 0, :], self.cmask_d[:, 0:1024]), (stg[:, 1, :], self.cmask_d[:, 1024:2048]),
                        (self.MISCF[:, :, :].rearrange("p a b -> p (a b)"), self.cmisc_d[:, 128:512]), (stg[:, 4, 0:128], self.cmisc_d[:, 0:128]),
                        (self.ROPE[:, :, :, :].rearrange("p a b c -> p (a b c)"), self.crope_d)],
                 [], [sr[0], sr[1], sr[4], self.Cr])
        self.dve(lambda e: e.tensor_copy(out=self.MASK[:, 0:8, :].rearrange("p a b -> p (a b)"), in_=stg[:, 0, :]), [sr[0]], [self.Cr])
        self.dve(lambda e: e.tensor_copy(out=self.MASK[:, 8:16, :].rearrange("p a b -> p (a b)"), in_=stg[:, 1, :]), [sr[1]], [self.Cr])
        self.dve(lambda e: e.tensor_copy(out=self.IDENT[:, :], in_=stg[:, 4, 0:128]), [sr[4]], [self.Cr])
        self.dve(lambda e: e.tensor_copy(out=self.CAUS[:, :], in_=self.MISCF[:, 0, :]), [self.Cr], [self.Cr])
        self.dve(lambda e: e.memset(self.NEGH[:, :], -0.5), [], [self.Cr])
        self.dve(lambda e: e.memset(self.AGT[:, :], 1.0), [], [self.AGTr])
        pairs = [(self.GT[:, l, :], self.gT_d[l]) for l in range(DEPTH)]
        pairs += [(self.WA2[0:16, l, :], self.wa2_d[l]) for l in range(DEPTH)]
        pairs += [(self.WA2[16:17, l, :], self.ba_d[l]) for l in range(DEPTH)]
        pairs += [(self.SGB[:, l, :], self.sgb_d[l]) for l in range(DEPTH)]
        pairs += [(stg[:, 2 + l, 0:512], self.sgw_d[l].rearrange("s g t -> s (g t)")) for l in range(DEPTH)]
        self.dma("c1", pairs, [], [self.Pr, sr[2], sr[3]])
        for l in range(DEPTH):
            self.dve(lambda e, l=l: e.tensor_tensor(
                out=self.SGW[:, l, :, :], in0=stg[:, 2 + l, 0:512].rearrange("p (g t) -> p g t", g=4),
                in1=self.MISCF[:, 0:1, :].to_broadcast([128, 4, 128]), op=ALU.mult), [sr[2 + l], self.Cr], [self.Pr])

    def convert_weights(self):
        jobs = []
        for l in range(self.nlayers):
            jobs.append((self.w_in_f[l], self.win_b[l], D * IN_DIM // 128, "cv_win%d" % l))
            jobs.append((self.w_out_f[l], self.wout_b[l], D * D // 128, "cv_wout%d" % l))
            jobs.append((self.w_ff1_f[l], self.wff1_b[l], D * DFF // 128, "cv_ff1%d" % l))
            jobs.append((self.w_ff2_f[l], self.wff2_b[l], DFF * D // 128, "cv_ff2%d" % l))
        self.wres = {}
        CH = 2048
        sin_ap = [self.X[:, 10 + 2 * i:12 + 2 * i, :].rearrange("p a b -> p (a b)") for i in range(2)]
        sin_r = [[self.Xr[10 + 2 * i], self.Xr[11 + 2 * i]] for i in range(2)]
        sout_ap = [self.X[:, 14 + i, :].bitcast(BF16) for i in range(2)]
        sout_r = [[self.Xr[14 + i]] for i in range(2)]
        k = 0
        engs = ["dve", "act", "pool"]
        for (src, dst, n, name) in jobs:
            sflat = src.rearrange("r c -> (r c)").rearrange("(p n) -> p n", p=128)
            dflat = dst.rearrange("r c -> (r c)").rearrange("(p n) -> p n", p=128)
            wr = self.res(name)
            self.wres[name] = wr
            off = 0
            while off < n:
                c = min(CH, n - off)
                i = k % 2
                si = sin_ap[i][:, 0:c]
                so = sout_ap[i][:, 0:c]
                self.dma("cvi%d" % i, [(si, sflat[:, off:off + c])], [], sin_r[i])
                en = engs[k % 3]
                if en == "act":
                    self.act(lambda e, so=so, si=si: e.activation(out=so, in_=si, func=AF.Copy), sin_r[i], sout_r[i])
                elif en == "dve":
                    self.dve(lambda e, so=so, si=si: e.tensor_copy(out=so, in_=si), sin_r[i], sout_r[i])
                else:
                    self.pool(lambda e, so=so, si=si: e.tensor_copy(out=so, in_=si), sin_r[i], sout_r[i])
                self.dma("cvo%d" % i, [(dflat[:, off:off + c], so)], sout_r[i], [wr])
                off += c
                k += 1

    def load_x(self, s):
        for b in range(NB):
            self.dma("X%d" % b, [(self.X[:, b, :], self.x_in[s, b * 128:(b + 1) * 128, :])], [], [self.Xr[b]])

    def store_x(self, s):
        for b in range(NB):
            r = self.res("y%d_%d" % (s, b))
            self.dma("X%d" % b, [(self.y_out[s, b * 128:(b + 1) * 128, :], self.X[:, b, :])], [self.Xr[b]], [r])
            self.out_res.append(r)

    def rms_to_T(self, b, gcol, l, dstT, dstTr, dst_cols):
        X, Xr = self.X, self.Xr
        st = self.ST
        HN, HNr = self.HNs[self.hni % 2], self.HNrs[self.hni % 2]
        self.hni += 1
        self.act(lambda e: e.activation(out=HN[:, :], in_=X[:, b, :], func=AF.Square, accum_out=st[:, 0:1]),
                 [Xr[b]], [HNr, self.STr[0]])
        self.pool(lambda e: e.tensor_scalar(out=st[:, 1:2], in0=st[:, 0:1], scalar1=1.0 / D, scalar2=EPS, op0=ALU.mult, op1=ALU.add),
                  [self.STr[0]], [self.STr[1]])
        self.pool(lambda e: e.tensor_tensor(out=st[:, 2:3], in0=st[:, 1:2], in1=self.NEGH[:, 0:1], op=ALU.pow),
                  [self.STr[1], self.Cr], [self.STr[2]])
        self.act(lambda e: e.activation(out=HN[:, :], in_=X[:, b, :], func=AF.Copy, scale=st[:, 2:3]),
                 [Xr[b], self.STr[2]], [HNr])
        k = self.bank()
        pst = self.PS[k].bitcast(BF16)

        def tr(e):
            ins = None
            for kc in range(8):
                ins = e.transpose(pst[:, kc * 128:(kc + 1) * 128], HN[:, kc * 128:(kc + 1) * 128], self.IDENT[:, :])
            return ins
        self.pe(tr, [HNr, self.Cr], [self.PSr[k]])
        self.dve(lambda e: e.tensor_tensor(out=dstT[:, :, dst_cols], in0=pst[:, :].rearrange("p (k t) -> p k t", k=8),
                                           in1=self.GT[:, l, gcol * 8:(gcol + 1) * 8].unsqueeze(2).to_broadcast([128, 8, 128]), op=ALU.mult),
                 [self.PSr[k], self.Pr], [dstTr])

    def proj(self, c0, ncols, reads_extra=()):
        k = self.bank()
        ps = self.PS[k]
        HT = self.HT

        def fn(e):
            ins = None
            for kc in range(8):
                ins = e.matmul(ps[:, 0:ncols], HT[:, kc, :], self.WIN[:, kc, c0:c0 + ncols], start=(kc == 0), stop=(kc == 7))
            return ins
        self.pe(fn, [self.HTr, self.WINr], [self.PSr[k]])
        return k

    def mixer_phase(self, s, l):
        self.P.phase_switch(self.ffnres, self.mixres)
        self.dma("win", [(self.WIN[:, :, :], self.win_b[l].rearrange("(k p) c -> p k c", p=128))],
                 [self.wres["cv_win%d" % l]], [self.WINr])
        self.dma("wout", [(self.WOUT, self.wout_b[l].rearrange("(k p) c -> p k c", p=128))],
                 [self.wres["cv_wout%d" % l]], [self.WOUTr])
        self.dma("bc", [(self.BC[:, :], self.bc_d[l])], [], [self.BCr])
        self.pool(lambda e: e.memset(self.VAUG[:, :, :, 64:66], 1.0), [], [self.VONESr])
        self.pool(lambda e: e.memset(self.SF[:, :], 0.0), [], [self.SFr])
        self.pool(lambda e: e.memset(self.SB_[:, :], 0.0), [], [self.SBr])
        nb = NB if self.stage >= 3 else 1
        for b in range(nb):
            self.mixer_block(s, l, b)

    def mixer_block(self, s, l, b):
        st = self.ST
        F = self.FAs[b % self.NF[0]]
        Fr = self.FArs[b % self.NF[0]]
        PS, PSr = self.PS, self.PSr
        tapit = (s == 0 and l == 0 and b == 0)
        self.HT, self.HTr = self.HTs[b % 2], self.HTrs[b % 2]
        self.CC, self.CCr = self.CCs[b % self.NCC], self.CCrs[b % self.NCC]
        self.rms_to_T(b, 0, l, self.HT, self.HTr, slice(0, 128))
        if tapit:
            self.tap("hT", self.HT[:, :, :].rearrange("p k t -> p (k t)"), self.HTr, 1024)
        kq = self.proj(OQ, 384)
        kk = self.proj(OK_, 384)
        self.act(lambda e: e.activation(out=F[0][:, 0:384], in_=PS[kq][:, 0:384], func=AF.Copy), [PSr[kq]], [Fr[0]])
        self.act(lambda e: e.activation(out=F[1][:, 0:384], in_=PS[kq][:, 0:384], func=AF.Square), [PSr[kq]], [Fr[1]])
        self.act(lambda e: e.activation(out=F[0][:, 384:768], in_=PS[kk][:, 0:384], func=AF.Copy), [PSr[kk]], [Fr[0]])
        self.act(lambda e: e.activation(out=F[1][:, 384:768], in_=PS[kk][:, 0:384], func=AF.Square), [PSr[kk]], [Fr[1]])
        self.dve(lambda e: e.tensor_reduce(out=st[:, 4:16], in_=F[1][:, :].rearrange("p (h c) -> p h c", c=64), axis=AX.X, op=ALU.add),
                 [Fr[1]], [self.STr[3]])
        self.pool(lambda e: e.tensor_scalar(out=st[:, 16:28], in0=st[:, 4:16], scalar1=1.0 / 64, scalar2=EPS, op0=ALU.mult, op1=ALU.add),
                  [self.STr[3]], [self.STr[4]])
        self.pool(lambda e: e.tensor_tensor(out=st[:, 28:40], in0=st[:, 16:28], in1=self.NEGH[:, 0:12], op=ALU.pow),
                  [self.STr[4], self.Cr], [self.STr[5]])
        self.dve(lambda e: e.tensor_tensor(out=F[1][:, :].rearrange("p (h c) -> p h c", c=64), in0=F[0][:, :].rearrange("p (h c) -> p h c", c=64),
                                           in1=st[:, 28:40].unsqueeze(2).to_broadcast([128, 12, 64]), op=ALU.mult),
                 [Fr[0], self.STr[5]], [Fr[1]])
        self.dve(lambda e: e.tensor_tensor(out=F[0][:, :].rearrange("p (a h c) -> p a h c", a=2, c=64),
                                           in0=F[1][:, :].rearrange("p (a h c) -> p a h c", a=2, c=64),
                                           in1=self.BC[:, 0:128].rearrange("p (a c) -> p a c", a=2).unsqueeze(2).to_broadcast([128, 2, 6, 64]),
                                           op=ALU.mult),
                 [Fr[1], self.BCr], [Fr[0]])
        self.act(lambda e: e.activation(out=self.QKN, in_=F[0][:, :], func=AF.Copy), [Fr[0]], [self.QKNr])
        x = F[0][:, :].rearrange("p (h c) -> p h c", c=64)
        x1, x2 = x[:, :, 0:8], x[:, :, 8:16]
        cosb = self.ROPE[:, 0, b:b + 1, :].to_broadcast([128, 12, 8])
        sinb = self.ROPE[:, 1, b:b + 1, :].to_broadcast([128, 12, 8])
        t = F[2][:, 0:384].rearrange("p (a h c) -> p a h c", a=4, c=8)
        qn = self.QKN.rearrange("p (h c) -> p h c", c=64)
        self.dve(lambda e: e.tensor_tensor(out=t[:, 0], in0=x1, in1=cosb, op=ALU.mult), [Fr[0], self.Cr], [Fr[2]])
        self.dve(lambda e: e.tensor_tensor(out=t[:, 1], in0=x2, in1=sinb, op=ALU.mult), [Fr[0], self.Cr], [Fr[2]])
        self.dve(lambda e: e.tensor_tensor(out=t[:, 2], in0=x2, in1=cosb, op=ALU.mult), [Fr[0], self.Cr], [Fr[2]])
        self.dve(lambda e: e.tensor_tensor(out=t[:, 3], in0=x1, in1=sinb, op=ALU.mult), [Fr[0], self.Cr], [Fr[2]])
        self.dve(lambda e: e.tensor_tensor(out=qn[:, :, 0:8], in0=t[:, 0], in1=t[:, 1], op=ALU.subtract), [Fr[2], self.QKNr], [self.QKNr])
        self.dve(lambda e: e.tensor_tensor(out=qn[:, :, 8:16], in0=t[:, 2], in1=t[:, 3], op=ALU.add), [Fr[2], self.QKNr], [self.QKNr])
        if tapit:
            self.tap("qkn", self.QKN, self.QKNr, 768)
        k = self.bank()
        pst = PS[k].bitcast(BF16)

        def trqk(e):
            ins = None
            for j in range(6):
                ins = e.transpose(pst[:, j * 128:(j + 1) * 128], self.QKN[:, j * 128:(j + 1) * 128], self.IDENT[:, :])
            return ins
        self.pe(trqk, [self.QKNr, self.Cr], [PSr[k]])
        QT, QTr = self.QT[b % 2], self.QTr[b % 2]
        self.act(lambda e: e.activation(out=QT, in_=pst[:, 0:384].rearrange("p (j t) -> p j t", j=3), func=AF.Copy),
                 [PSr[k]], [QTr])
        self.act(lambda e: e.activation(out=self.KT[:, :, b * 128:(b + 1) * 128], in_=pst[:, 384:768].rearrange("p (j t) -> p j t", j=3), func=AF.Copy),
                 [PSr[k]], [self.QKTr[b]])
        kv = self.proj(OV, 384)
        self.act(lambda e: e.activation(out=self.VAUG[:, b, :, 0:64], in_=PS[kv][:, 0:384].rearrange("p (h c) -> p h c", c=64), func=AF.Copy),
                 [PSr[kv]], [self.VAUGr[b]])
        if self.stage < 2:
            return
        self.attention(b, tapit)
        if self.stage < 4:
            return
        self.gla(l, b, tapit)
        if self.stage < 5:
            return
        self.sg(l, b, tapit)
        if self.stage < 6:
            return
        self.outproj(b, tapit)

    def attention(self, b, tapit):
        CC, CCr = self.CC, self.CCr
        QT, QTr = self.QT[b % 2], self.QTr[b % 2]
        PS, PSr = self.PS, self.PSr
        ei = 0
        groups = [list(range(g, min(g + 4, b + 1))) for g in range(0, b + 1, 4)]
        for pr in range(3):
            heads = (2 * pr, 2 * pr + 1)
            for kbs in groups:
                n = len(kbs)
                ks = (self.bank(), self.bank())

                def qk(e, kbs=kbs, ks=ks, pr=pr):
                    ins = None
                    for i, kb in enumerate(kbs):
                        for half in range(2):
                            rows = slice(64 * half, 64 * half + 64)
                            ins = e.matmul(PS[ks[half]][:, i * 128:(i + 1) * 128], self.KT[rows, pr, kb * 128:(kb + 1) * 128],
                                           QT[rows, pr, :], start=True, stop=True)
                    return ins
                self.pe(qk, [self.QKTr[kb] for kb in kbs] + [QTr], [PSr[ks[0]], PSr[ks[1]]])
                j0 = 15 - b + kbs[0]
                for half in range(2):
                    h = heads[half]
                    k = ks[half]
                    ko = 6 + half
                    E, Er = self.E[ei % self.NE], self.Er[ei % self.NE]
                    ei += 1
                    self.act(lambda e, E=E, k=k, n=n: e.activation(out=E[:, 0:n * 128], in_=PS[k][:, 0:n * 128], func=AF.Exp, scale=0.125),
                             [PSr[k]], [Er])
                    mfn = (lambda e, E=E, n=n, j0=j0: e.tensor_tensor(out=E[:, 0:n * 128], in0=E[:, 0:n * 128],
                                                                      in1=self.MASK[:, j0:j0 + n, :].rearrange("p a b -> p (a b)"), op=ALU.mult))
                    if ei % 3 == 0:
                        self.pool(mfn, [Er, self.Cr], [Er])
                    else:
                        self.dve(mfn, [Er, self.Cr], [Er])

                    def pv(e, kbs=kbs, E=E, h=h, ko=ko):
                        ins = None
                        for i, kb in enumerate(kbs):
                            ins = e.matmul(PS[ko][:, 0:65], E[:, i * 128:(i + 1) * 128], self.VAUG[:, kb, h, 0:65],
                                           start=(kb == 0), stop=(kb == b))
                        return ins
                    self.pe(pv, [Er, self.VONESr] + [self.VAUGr[kb] for kb in kbs], [PSr[ko]])
            for half in range(2):
                h = heads[half]
                ko = 6 + half
                sr = self.STr[6 + half]
                sc = self.ST[:, 40 + half:41 + half]
                self.dve(lambda e, sc=sc, ko=ko: e.reciprocal(out=sc, in_=PS[ko][:, 64:65]), [PSr[ko]], [sr])
                self.dve(lambda e, sc=sc, ko=ko, h=h: e.tensor_scalar(out=CC[:, h * 64:(h + 1) * 64], in0=PS[ko][:, 0:64], scalar1=sc, scalar2=None,
                                                                      op0=ALU.mult), [PSr[ko], sr], [CCr[0]])
        if tapit:
            self.tap("att", CC[:, 0:384], CCr[0], 384)

    def gla(self, l, b, tapit):
        CC, CCr = self.CC, self.CCr
        PS, PSr = self.PS, self.PSr
        F, Fr = self.FGs[b % self.NF[1]], self.FGrs[b % self.NF[1]]
        st = self.ST
        kqk = self.proj(OQG, 384)
        self.act(lambda e: e.activation(out=F[0][:, 0:384], in_=PS[kqk][:, 0:384], func=AF.Copy), [PSr[kqk]], [Fr[0]])
        kvg = self.proj(OVG, 384)
        self.act(lambda e: e.activation(out=self.VG[:, :], in_=PS[kvg][:, 0:384], func=AF.Copy), [PSr[kvg]], [self.VGr])
        krg = self.proj(ORG, 384)
        self.act(lambda e: e.activation(out=F[1][:, 0:384], in_=PS[krg][:, 0:384], func=AF.Copy), [PSr[krg]], [Fr[1]])
        self.act(lambda e: e.activation(out=F[1][:, 384:768], in_=PS[krg][:, 0:384], func=AF.Tanh, scale=0.5), [PSr[krg]], [Fr[1]])
        ka = self.bank()

        HT = self.HT

        def fa(e):
            ins = None
            for kc in range(8):
                ins = e.matmul(PS[ka][0:16, 0:128], self.WIN[:, kc, OAG:OAG + 16], HT[:, kc, :], start=(kc == 0), stop=(kc == 7))
            return ins
        self.pe(fa, [self.HTr, self.WINr], [PSr[ka]])
        self.act(lambda e: e.activation(out=self.AGT[0:16, :], in_=PS[ka][0:16, 0:128], func=AF.Copy), [PSr[ka], self.AGTr], [self.AGTr])
        kg = self.bank()

        def fg(e):
            return e.matmul(PS[kg][:, 0:192], self.AGT[0:17, :], self.WA2[:, l, :], start=True, stop=True)
        self.pe(fg, [self.AGTr, self.Pr, self.Cr], [PSr[kg]])
        self.act(lambda e: e.activation(out=F[2][:, 0:192], in_=PS[kg][:, 0:192], func=AF.Exp, scale=-1.0), [PSr[kg]], [Fr[2]])
        self.act(lambda e: e.activation(out=F[2][:, 192:384], in_=F[2][:, 0:192], func=AF.Ln, bias=1.0), [Fr[2]], [Fr[2]])
        lneg = F[2][:, 192:384]
        kc_ = self.bank()

        def fc(e):
            e.matmul(PS[kc_][:, 0:192], self.MISCF[:, 0, :], lneg, start=True, stop=True)
            return e.matmul(PS[kc_][:, 192:384], self.MISCF[:, 1, :], lneg, start=True, stop=True)
        self.pe(fc, [Fr[2], self.Cr], [PSr[kc_]])
        kt = self.bank()

        def ftot(e):
            ins = None
            for h in range(6):
                ins = e.matmul(PS[kt][0:32, h:h + 1], F[2][:, 192 + h * 32:192 + (h + 1) * 32], self.MISCF[:, 2, 0:1], start=True, stop=True)
            return ins
        self.pe(ftot, [Fr[2], self.Cr], [PSr[kt]])
        self.act(lambda e: e.activation(out=self.DEC[:, :], in_=PS[kt][0:32, 0:6], func=AF.Exp, scale=-1.0 / 16), [PSr[kt]], [self.DECr])
        self.act(lambda e: e.activation(out=F[3][:, 0:384], in_=PS[kc_][:, 0:384], func=AF.Exp, scale=-1.0 / 16), [PSr[kc_]], [Fr[3]])
        self.act(lambda e: e.activation(out=F[3][:, 384:576], in_=PS[kc_][:, 0:192], func=AF.Exp, scale=1.0 / 16), [PSr[kc_]], [Fr[3]])
        self.dve(lambda e: e.scalar_tensor_tensor(out=self.GQK[:, 0:192], in0=F[0][:, 0:192], scalar=32.0 ** -0.5, in1=F[3][:, 0:192],
                                                  op0=ALU.mult, op1=ALU.mult), [Fr[0], Fr[3]], [self.GQKr])
        self.dve(lambda e: e.tensor_tensor(out=self.GQK[:, 192:384], in0=F[0][:, 192:384], in1=F[3][:, 384:576], op=ALU.mult),
                 [Fr[0], Fr[3]], [self.GQKr])
        self.dve(lambda e: e.tensor_tensor(out=self.GQK[:, 384:576], in0=F[0][:, 192:384], in1=F[3][:, 192:384], op=ALU.mult),
                 [Fr[0], Fr[3]], [self.GQKr])
        k1 = self.bank()
        k2 = self.bank()
        p1 = PS[k1].bitcast(BF16)
        p2 = PS[k2].bitcast(BF16)

        def ftr(e):
            ins = None
            for h in range(6):
                e.transpose(p1[0:32, h * 128:(h + 1) * 128], self.GQK[:, h * 32:(h + 1) * 32], self.IDENT[:, :])
                ins = e.transpose(p2[0:32, h * 128:(h + 1) * 128], self.GQK[:, 192 + h * 32:192 + (h + 1) * 32], self.IDENT[:, :])
            return ins
        self.pe(ftr, [self.GQKr, self.Cr], [PSr[k1], PSr[k2]])
        self.act(lambda e: e.activation(out=self.GT2[:, 0:6, :], in_=p1[0:32, 0:768].rearrange("p (h t) -> p h t", h=6), func=AF.Copy),
                 [PSr[k1]], [self.GT2r])
        self.act(lambda e: e.activation(out=self.GT2[:, 6:12, :], in_=p2[0:32, 0:768].rearrange("p (h t) -> p h t", h=6), func=AF.Copy),
                 [PSr[k2]], [self.GT2r])
        ka1 = self.bank()
        ka2 = self.bank()

        def fA(e):
            ins = None
            for h in range(6):
                ps = PS[ka1] if h < 4 else PS[ka2]
                c = (h % 4) * 128
                ins = e.matmul(ps[:, c:c + 128], self.GT2[:, 6 + h, :], self.GT2[:, h, :], start=True, stop=True)
            return ins
        self.pe(fA, [self.GT2r], [PSr[ka1], PSr[ka2]])
        self.dve(lambda e: e.tensor_tensor(out=self.AM[:, 0:512].rearrange("p (h t) -> p h t", h=4), in0=PS[ka1][:, 0:512].rearrange("p (h t) -> p h t", h=4),
                                           in1=self.CAUS[:, :].unsqueeze(1).to_broadcast([128, 4, 128]), op=ALU.mult),
                 [PSr[ka1], self.Cr], [self.AMr])
        self.dve(lambda e: e.tensor_tensor(out=self.AM[:, 512:768].rearrange("p (h t) -> p h t", h=2), in0=PS[ka2][:, 0:256].rearrange("p (h t) -> p h t", h=2),
                                           in1=self.CAUS[:, :].unsqueeze(1).to_broadcast([128, 2, 128]), op=ALU.mult),
                 [PSr[ka2], self.Cr], [self.AMr])
        ko = self.bank()

        def fo(e):
            ins = None
            for h in range(6):
                e.matmul(PS[ko][:, h * 64:(h + 1) * 64], self.AM[:, h * 128:(h + 1) * 128], self.VG[:, h * 64:(h + 1) * 64], start=True, stop=False)
                ins = e.matmul(PS[ko][:, h * 64:(h + 1) * 64], self.GT2[:, h, :], self.SB_[:, h * 64:(h + 1) * 64], start=False, stop=True)
            return ins
        self.pe(fo, [self.AMr, self.VGr, self.GT2r, self.SBr], [PSr[ko]])
        ks = self.bank()

        def fkv(e):
            ins = None
            for h in range(6):
                ins = e.matmul(PS[ks][0:32, h * 64:(h + 1) * 64], self.GQK[:, 384 + h * 32:384 + (h + 1) * 32], self.VG[:, h * 64:(h + 1) * 64],
                               start=True, stop=True)
            return ins
        self.pe(fkv, [self.GQKr, self.VGr], [PSr[ks]])
        self.dve(lambda e: e.tensor_tensor(out=self.SF[:, :].rearrange("p (h v) -> p h v", h=6), in0=self.SF[:, :].rearrange("p (h v) -> p h v", h=6),
                                           in1=self.DEC[:, :].unsqueeze(2).to_broadcast([32, 6, 64]), op=ALU.mult),
                 [self.SFr, self.DECr], [self.SFr])
        self.dve(lambda e: e.tensor_tensor(out=self.SF[:, :], in0=PS[ks][0:32, 0:384], in1=self.SF[:, :], op=ALU.add),
                 [PSr[ks], self.SFr], [self.SFr])
        self.dve(lambda e: e.tensor_copy(out=self.SB_[:, :], in_=self.SF[:, :]), [self.SFr], [self.SBr])
        self.act(lambda e: e.activation(out=F[4][:, 0:384], in_=PS[ko][:, 0:384], func=AF.Square), [PSr[ko]], [Fr[4]])
        self.dve(lambda e: e.tensor_reduce(out=st[:, 44:50], in_=F[4][:, 0:384].rearrange("p (h c) -> p h c", c=64), axis=AX.X, op=ALU.add),
                 [Fr[4]], [self.STr[13]])
        self.pool(lambda e: e.tensor_scalar(out=st[:, 50:56], in0=st[:, 44:50], scalar1=1.0 / 64, scalar2=EPS, op0=ALU.mult, op1=ALU.add),
                  [self.STr[13]], [self.STr[14]])
        self.pool(lambda e: e.tensor_tensor(out=st[:, 56:62], in0=st[:, 50:56], in1=self.NEGH[:, 0:6], op=ALU.pow),
                  [self.STr[14], self.Cr], [self.STr[15]])
        self.dve(lambda e: e.scalar_tensor_tensor(out=F[4][:, 384:768], in0=F[1][:, 384:768], scalar=1.0, in1=F[1][:, 0:384], op0=ALU.add, op1=ALU.mult),
                 [Fr[1]], [Fr[4]])
        self.dve(lambda e: e.tensor_tensor(out=F[4][:, 0:384].rearrange("p (h c) -> p h c", c=64), in0=PS[ko][:, 0:384].rearrange("p (h c) -> p h c", c=64),
                                           in1=st[:, 56:62].unsqueeze(2).to_broadcast([128, 6, 64]), op=ALU.mult),
                 [PSr[ko], self.STr[15], Fr[4]], [Fr[4]])
        self.dve(lambda e: e.tensor_tensor(out=F[4][:, 0:384], in0=F[4][:, 0:384], in1=F[4][:, 384:768], op=ALU.mult), [Fr[4]], [Fr[4]])
        self.dve(lambda e: e.scalar_tensor_tensor(out=CC[:, 384:768].rearrange("p (h c) -> p h c", c=64), in0=F[4][:, 0:384].rearrange("p (h c) -> p h c", c=64),
                                                  scalar=0.5, in1=self.BC[:, 128:192].unsqueeze(1).to_broadcast([128, 6, 64]),
                                                  op0=ALU.mult, op1=ALU.mult), [Fr[4], self.BCr], [CCr[1]])
        if tapit:
            self.tap("gla", CC[:, 384:768], CCr[1], 384)

    def sg(self, l, b, tapit):
        CC, CCr = self.CC, self.CCr
        PS, PSr = self.PS, self.PSr
        S0, S1 = self.FSs[b % self.NF[2]]
        S0r, S1r = self.FSrs[b % self.NF[2]]
        st = self.ST
        kz = self.proj(OZS, 512)
        z = PS[kz][:, 0:512]
        C0 = 0.7978845608028654
        self.act(lambda e: e.activation(out=S1[:, :], in_=z, func=AF.Copy), [PSr[kz]], [S1r])
        self.act(lambda e: e.activation(out=S0[:, :], in_=z, func=AF.Square), [PSr[kz]], [S0r])
        self.dve(lambda e: e.tensor_scalar(out=S0[:, :], in0=S0[:, :], scalar1=0.044715, scalar2=1.0, op0=ALU.mult, op1=ALU.add),
                 [S0r], [S0r])
        self.dve(lambda e: e.tensor_tensor(out=S0[:, :], in0=S1[:, :], in1=S0[:, :], op=ALU.mult), [S1r, S0r], [S0r])
        self.act(lambda e: e.activation(out=S0[:, :], in_=S0[:, :], func=AF.Tanh, scale=C0), [S0r], [S0r])
        self.dve(lambda e: e.scalar_tensor_tensor(out=S1[:, :], in0=S0[:, :], scalar=1.0, in1=S1[:, :], op0=ALU.add, op1=ALU.mult),
                 [S0r, S1r], [S1r])
        u2 = S1[:, 0:256]
        v2 = S1[:, 256:512]
        self.dve(lambda e: e.tensor_reduce(out=st[:, 62:63], in_=v2, axis=AX.X, op=ALU.add), [S1r], [self.STr[8]])
        self.pool(lambda e: e.tensor_scalar(out=st[:, 63:64], in0=st[:, 62:63], scalar1=-1.0 / 256, scalar2=None, op0=ALU.mult),
                  [self.STr[8]], [self.STr[9]])
        self.act(lambda e: e.activation(out=S0[:, 0:256], in_=v2, func=AF.Identity, bias=st[:, 63:64]), [S1r, self.STr[9], S0r], [S0r])
        self.act(lambda e: e.activation(out=S0[:, 256:512], in_=S0[:, 0:256], func=AF.Square, accum_out=st[:, 3:4]), [S0r], [S0r, self.STr[10]])
        self.pool(lambda e: e.tensor_scalar(out=st[:, 42:43], in0=st[:, 3:4], scalar1=1.0 / 256, scalar2=4.0 * EPS, op0=ALU.mult, op1=ALU.add),
                  [self.STr[10]], [self.STr[11]])
        self.pool(lambda e: e.tensor_tensor(out=st[:, 43:44], in0=st[:, 42:43], in1=self.NEGH[:, 0:1], op=ALU.pow),
                  [self.STr[11], self.Cr], [self.STr[12]])
        self.dve(lambda e: e.scalar_tensor_tensor(out=S0[:, 256:512], in0=S0[:, 0:256], scalar=st[:, 43:44], in1=self.BC[:, 192:448],
                                                  op0=ALU.mult, op1=ALU.mult), [S0r, self.STr[12], self.BCr], [S0r])
        self.dve(lambda e: e.tensor_tensor(out=self.VLN[:, :], in0=S0[:, 256:512], in1=self.BC[:, 448:704], op=ALU.add),
                 [S0r, self.BCr], [self.VLNr])
        ks = self.bank()

        def fs(e):
            ins = None
            for g in range(4):
                ins = e.matmul(PS[ks][:, g * 64:(g + 1) * 64], self.SGW[:, l, g, :], self.VLN[:, g * 64:(g + 1) * 64], start=True, stop=True)
            return ins
        self.pe(fs, [self.VLNr, self.Pr], [PSr[ks]])
        self.dve(lambda e: e.tensor_tensor(out=S0[:, 0:256].rearrange("p (g c) -> p g c", g=4), in0=PS[ks][:, 0:256].rearrange("p (g c) -> p g c", g=4),
                                           in1=self.SGB[:, l, :].unsqueeze(2).to_broadcast([128, 4, 64]), op=ALU.add),
                 [PSr[ks], self.Pr, S0r], [S0r])
        self.dve(lambda e: e.scalar_tensor_tensor(out=CC[:, 768:1024], in0=u2, scalar=0.5, in1=S0[:, 0:256], op0=ALU.mult, op1=ALU.mult),
                 [S1r, S0r], [CCr[2]])
        if tapit:
            self.tap("sg", CC[:, 768:1024], CCr[2], 256)

    def outproj(self, b, tapit):
        CC, CCr = self.CC, self.CCr
        PS, PSr = self.PS, self.PSr
        k = self.bank()
        pst = PS[k].bitcast(BF16)

        def tr(e):
            ins = None
            for kc in range(8):
                ins = e.transpose(pst[:, kc * 128:(kc + 1) * 128], CC[:, kc * 128:(kc + 1) * 128], self.IDENT[:, :])
            return ins
        self.pe(tr, CCr + [self.Cr], [PSr[k]])
        self.act(lambda e: e.activation(out=self.CT, in_=pst[:, :].rearrange("p (k t) -> p k t", k=8), func=AF.Copy), [PSr[k]], [self.CTr])
        for n in range(2):
            k2 = self.bank()

            def mm(e, k2=k2, n=n):
                ins = None
                for kc in range(8):
                    ins = e.matmul(PS[k2][:, :], self.CT[:, kc, :], self.WOUT[:, kc, n * 512:(n + 1) * 512], start=(kc == 0), stop=(kc == 7))
                return ins
            self.pe(mm, [self.CTr, self.WOUTr], [PSr[k2]])
            self.dve(lambda e, k2=k2, n=n: e.tensor_tensor(out=self.X[:, b, n * 512:(n + 1) * 512], in0=PS[k2][:, :], in1=self.X[:, b, n * 512:(n + 1) * 512], op=ALU.add),
                     [PSr[k2], self.Xr[b]], [self.Xr[b]])

    def ffn_phase(self, s, l):
        PS, PSr = self.PS, self.PSr
        TT = self.TT
        nblk = TT // 128
        w1 = self.wff1_b[l].rearrange("(k p) c -> p k c", p=128)
        w2 = self.wff2_b[l].rearrange("(k p) c -> p k c", p=128)
        ntile = S // TT
        self.P.phase_switch(self.mixres, self.ffnres)
        items = [(t, j) for t in range(ntile) for j in range(8)]

        def norms(t):
            H2T, H2Tr = self.H2Ts[t % 2], self.H2Trs[t % 2]
            for i in range(nblk):
                self.rms_to_T(t * nblk + i, 1, l, H2T, H2Tr, slice(i * 128, (i + 1) * 128))

        def ff1(idx):
            t, j = items[idx]
            sl = idx % 2
            H2T, H2Tr = self.H2Ts[t % 2], self.H2Trs[t % 2]
            self.dma("w1_%d" % sl, [(self.W1[sl], w1[:, :, j * 512:(j + 1) * 512])], [self.wres["cv_ff1%d" % l]], [self.W1r[sl]])
            H1, H1r = self.H1T[sl], self.H1Tr[sl]
            for fc in range(4):
                for c0 in range(0, TT, 512):
                    k = self.bank()

                    def f1(e, k=k, fc=fc, sl=sl, c0=c0, H2T=H2T):
                        ins = None
                        for kc in range(8):
                            ins = e.matmul(PS[k][:, :], self.W1[sl][:, kc, fc * 128:(fc + 1) * 128], H2T[:, kc, c0:c0 + 512],
                                           start=(kc == 0), stop=(kc == 7))
                        return ins
                    self.pe(f1, [self.W1r[sl], H2Tr], [PSr[k]])
                    ri = (fc + c0 // 512) % 2
                    self.act(lambda e, k=k, ri=ri: e.activation(out=self.RELU[ri][:, 0:512], in_=PS[k][:, :], func=AF.Relu), [PSr[k]], [self.RELUr[ri]])
                    self.pool(lambda e, ri=ri, fc=fc, H1=H1, c0=c0: e.tensor_tensor(out=H1[:, fc, c0:c0 + 512], in0=self.RELU[ri][:, 0:512], in1=self.RELU[ri][:, 0:512], op=ALU.mult),
                              [self.RELUr[ri]], [H1r])

        def ff2(idx):
            t, j = items[idx]
            sl = idx % 2
            self.dma("w2_%d" % sl, [(self.W2[sl], w2[:, j * 4:(j + 1) * 4, :])], [self.wres["cv_ff2%d" % l]], [self.W2r[sl]])
            H1, H1r = self.H1T[sl], self.H1Tr[sl]
            for i in range(nblk):
                b = t * nblk + i
                for n in range(2):
                    k = self.bank()

                    def f2(e, k=k, i=i, n=n, sl=sl, H1=H1):
                        ins = None
                        for fc in range(4):
                            ins = e.matmul(PS[k][:, :], H1[:, fc, i * 128:(i + 1) * 128], self.W2[sl][:, fc, n * 512:(n + 1) * 512],
                                           start=(fc == 0), stop=(fc == 3))
                        return ins
                    self.pe(f2, [H1r, self.W2r[sl]], [PSr[k]])
                    self.dve(lambda e, k=k, b=b, n=n: e.tensor_tensor(out=self.X[:, b, n * 512:(n + 1) * 512], in0=PS[k][:, :],
                                                                       in1=self.X[:, b, n * 512:(n + 1) * 512], op=ALU.add),
                             [PSr[k], self.Xr[b]], [self.Xr[b]])

        norms(0)
        for idx in range(len(items)):
            t, j = items[idx]
            if j == 4 and t + 1 < ntile:
                norms(t + 1)
            ff1(idx)
            if idx > 0:
                ff2(idx - 1)
        ff2(len(items) - 1)


def _constants():
    k = np.arange(128)[:, None]
    q = np.arange(128)[None, :]
    mask = np.zeros((128, 16, 128), np.float32)
    for j in range(16):
        delta = 15 - j
        dist = 128 * delta + q - k
        m1 = (dist >= 0) & (dist <= 128)
        m4 = (dist % 4 == 0) & (dist >= 0) & (dist <= 512)
        m16 = (dist % 16 == 0) & (dist >= 0) & (dist <= 2048)
        mask[:, j, :] = m1.astype(np.float32) + m4.astype(np.float32) + m16.astype(np.float32)
    ident = np.eye(128, dtype=np.float32)
    causal = (k <= q).astype(np.float32)
    triu = (k > q).astype(np.float32)
    ones = np.ones((128, 128), np.float32)
    misc = np.concatenate([ident, causal, triu, ones], axis=1)
    inv = (500000.0 ** (-np.arange(0, 16, 2, dtype=np.float32) / 16.0)).astype(np.float32)
    pos = (np.arange(16)[None, :] * 128 + np.arange(128)[:, None]).astype(np.float32)
    ang = pos[:, :, None] * inv[None, None, :]
    rope = np.concatenate([np.cos(ang).reshape(128, 128), np.sin(ang).reshape(128, 128)], axis=1).astype(np.float32)
    return mask.reshape(128, 2048), misc, rope


def _host_inputs(inp):
    f = lambda a: np.ascontiguousarray(np.asarray(a, dtype=np.float32))
    mask, misc, rope = _constants()
    n1, n2 = f(inp["norm1_g"]), f(inp["norm2_g"])
    gT = np.concatenate([n1.reshape(DEPTH, 8, 128).transpose(0, 2, 1), n2.reshape(DEPTH, 8, 128).transpose(0, 2, 1)], axis=2)
    row = np.concatenate([f(inp["q_norm_g"]), f(inp["k_norm_g"]), f(inp["gla_norm_g"]), f(inp["sg_ln_g"]), f(inp["sg_ln_b"])], axis=1)
    bc = np.broadcast_to(row[:, None, :], (DEPTH, 128, 704))
    common = {
        "w_in": f(inp["w_in"]), "w_out": f(inp["w_out"]), "w_ff1": f(inp["w_ff1"]), "w_ff2": f(inp["w_ff2"]),
        "gT": f(gT), "bc": f(bc), "wa2": f(inp["gla_w_a2"]), "ba": f(inp["gla_b_a"]).reshape(DEPTH, 1, 192),
        "sgwT": f(np.transpose(f(inp["sg_w"]), (0, 3, 1, 2))), "sgbT": f(np.transpose(f(inp["sg_b"]), (0, 2, 1))),
        "c_mask": mask, "c_misc": misc, "c_rope": rope,
    }
    return common


_NC_CACHE = {}


def kernel(**inputs):
    x = np.ascontiguousarray(np.asarray(inputs["x"], dtype=np.float32))
    common = _host_inputs(inputs)
    if "nc" not in _NC_CACHE:
        _NC_CACHE["nc"] = Builder().build()
    nc = _NC_CACHE["nc"]
    in_maps = []
    for c in range(NCORES):
        m = dict(common)
        m["x"] = x[c * SEQ_PER_CORE:(c + 1) * SEQ_PER_CORE]
        in_maps.append(m)
    res = run_bass_kernel_spmd(nc, in_maps, core_ids=list(range(NCORES)))
    out = np.concatenate([np.asarray(r["y"]) for r in res.results], axis=0)
    return out.astype(np.float32)
```
